# Optimizing a Trainium2 kernel written in Bass

```python
import jax, jax.numpy as jnp
from jax import lax
import numpy as np

D_MODEL = 2048
BATCH = 8
SEQ = 4096
DEPTH = 2

MLA_HEADS = 8
MLA_NOPE = 128
MLA_ROPE = 64
MLA_QK = MLA_NOPE + MLA_ROPE
MLA_V = 128
MLA_Q_RANK = 512
MLA_KV_RANK = 512
MLA_WIDTH = MLA_HEADS * MLA_V
ROPE_THETA = 10000.0
Q_BLOCK = 128
SGU_GROUPS = 8
SGU_CHUNK = 128
SGU_WIDTH = 1024
SGU_GROUP_DIM = SGU_WIDTH // SGU_GROUPS
GLA_HEADS = 4
GLA_DK = 128
GLA_DV = 256
GLA_GATE_RANK = 16
GLA_TAU = 16.0
GLA_CHUNK = 128
GLA_WIDTH = GLA_HEADS * GLA_DV
N_BRANCH = 3
BRANCH_WIDTH = 1024
EPS = 1e-6

IN_SIZES = (MLA_Q_RANK, MLA_KV_RANK, MLA_ROPE, MLA_WIDTH,
            SGU_WIDTH, SGU_WIDTH, SGU_WIDTH,
            GLA_HEADS * GLA_DK, GLA_HEADS * GLA_DK, GLA_WIDTH, GLA_GATE_RANK, GLA_WIDTH,
            N_BRANCH * D_MODEL)
IN_COLS = sum(IN_SIZES)
IN_SPLITS = tuple(int(c) for c in np.cumsum(IN_SIZES)[:-1])

kernel_name = "hybrid_mla_sgu_gla_gated_merge"


def rms_norm(x, g):
    xf = x.astype(jnp.float32)
    y = xf * lax.rsqrt(jnp.mean(xf * xf, axis=-1, keepdims=True) + EPS)
    return (y * g.astype(jnp.float32)).astype(x.dtype)


def rope_tables(positions, dtype):
    inv_freq = 1.0 / (ROPE_THETA ** (jnp.arange(0, MLA_ROPE, 2, dtype=jnp.float32) / MLA_ROPE))
    ang = positions.astype(jnp.float32)[..., None] * inv_freq
    return jnp.cos(ang).astype(dtype)[:, :, None, :], jnp.sin(ang).astype(dtype)[:, :, None, :]


def apply_rope(x, cos, sin):
    half = x.shape[-1] // 2
    x1, x2 = x[..., :half], x[..., half:]
    return jnp.concatenate([x1 * cos - x2 * sin, x2 * cos + x1 * sin], axis=-1)


def causal_block_attention(q, k, v):
    S = q.shape[1]
    scale = MLA_QK ** -0.5
    neg = jnp.finfo(jnp.float32).min
    outs = []
    for i in range(S // Q_BLOCK):
        q0, q1 = i * Q_BLOCK, (i + 1) * Q_BLOCK
        qb, kb, vb = q[:, q0:q1], k[:, :q1], v[:, :q1]
        s = jnp.einsum('bqhd,bkhd->bhqk', qb, kb).astype(jnp.float32) * scale
        mask = (q0 + jnp.arange(Q_BLOCK))[:, None] >= jnp.arange(q1)[None, :]
        p = jax.nn.softmax(jnp.where(mask, s, neg), axis=-1).astype(v.dtype)
        outs.append(jnp.einsum('bhqk,bkhd->bqhd', p, vb))
    return jnp.concatenate(outs, axis=1)


def mla_branch(c_q, c_kv, k_rope, cq_g, ckv_g, w_uq, w_ukv, q_g, k_g, cos, sin):
    B, S, _ = c_q.shape
    q = (rms_norm(c_q, cq_g) @ w_uq).reshape(B, S, MLA_HEADS, MLA_QK)
    kv = (rms_norm(c_kv, ckv_g) @ w_ukv).reshape(B, S, MLA_HEADS, MLA_NOPE + MLA_V)
    k_nope, v = kv[..., :MLA_NOPE], kv[..., MLA_NOPE:]
    k = jnp.concatenate([k_nope, jnp.broadcast_to(k_rope[:, :, None, :], (B, S, MLA_HEADS, MLA_ROPE))], axis=-1)
    q = rms_norm(q, q_g)
    k = rms_norm(k, k_g)
    q = jnp.concatenate([q[..., :MLA_NOPE], apply_rope(q[..., MLA_NOPE:], cos, sin)], axis=-1)
    k = jnp.concatenate([k[..., :MLA_NOPE], apply_rope(k[..., MLA_NOPE:], cos, sin)], axis=-1)
    o = causal_block_attention(q, k, v)
    return o.reshape(B, S, MLA_WIDTH)


def sgu_branch(u, v, v_g, w_s, b_s):
    B, S, _ = u.shape
    nc = S // SGU_CHUNK
    v = rms_norm(v, v_g).reshape(B, nc, SGU_CHUNK, SGU_GROUPS, SGU_GROUP_DIM)
    mix = jnp.einsum('gts,bcsgd->bctgd', jnp.tril(w_s), v) + b_s.T[:, :, None]
    return u * mix.reshape(B, S, SGU_WIDTH)


def gla_branch(q, k, v, a_r, w_a2, b_a, o_g):
    B, S, _ = q.shape
    nc = S // GLA_CHUNK
    L = GLA_CHUNK
    log_a = jax.nn.log_sigmoid((a_r @ w_a2 + b_a).astype(jnp.float32)) / GLA_TAU

    def chunks(t, d):
        return t.reshape(B, nc, L, GLA_HEADS, d).transpose(1, 0, 3, 2, 4)

    qc = chunks(q.astype(jnp.float32) * GLA_DK ** -0.5, GLA_DK)
    kc = chunks(k.astype(jnp.float32), GLA_DK)
    vc = chunks(v.astype(jnp.float32), GLA_DV)
    bc = jnp.cumsum(chunks(log_a, GLA_DK), axis=3)
    mask = jnp.tril(jnp.ones((L, L), dtype=bool))[:, :, None]

    def step(state, inp):
        q_, k_, v_, b_ = inp
        o_inter = jnp.einsum('bhtk,bhkv->bhtv', q_ * jnp.exp(b_), state)
        diff = b_[:, :, :, None, :] - b_[:, :, None, :, :]
        decay = jnp.where(mask, jnp.exp(jnp.where(mask, diff, 0.0)), 0.0)
        attn = jnp.einsum('bhtk,bhtsk,bhsk->bhts', q_, decay, k_)
        o = o_inter + jnp.einsum('bhts,bhsv->bhtv', attn, v_)
        b_last = b_[:, :, -1:, :]
        new_state = jnp.exp(b_last[:, :, 0, :])[..., None] * state + \
            jnp.einsum('bhsk,bhsv->bhkv', k_ * jnp.exp(b_last - b_), v_)
        return new_state, o

    state0 = jnp.zeros((B, GLA_HEADS, GLA_DK, GLA_DV), jnp.float32)
    _, o = lax.scan(step, state0, (qc, kc, vc, bc))
    o = o.transpose(1, 0, 3, 2, 4).reshape(B, S, GLA_HEADS, GLA_DV)
    o = rms_norm(o, o_g).astype(v.dtype)
    return o.reshape(B, S, GLA_WIDTH)


def hybrid_layer(x, cos, sin, norm_g, w_in, cq_g, ckv_g, w_uq, w_ukv, q_g, k_g,
                 v_g, w_s, b_s, w_a2, b_a, o_g, w_branch, w_out):
    B, S, D = x.shape
    h = rms_norm(x, norm_g)
    proj = h @ w_in
    (c_q, c_kv, k_rope, z_a, u_b, v_b, z_b, q_c, k_c, v_c, a_r, z_c, gate_logits) = \
        jnp.split(proj, IN_SPLITS, axis=-1)
    y_a = mla_branch(c_q, c_kv, k_rope, cq_g, ckv_g, w_uq, w_ukv, q_g, k_g, cos, sin) * jax.nn.silu(z_a)
    y_b = sgu_branch(jax.nn.gelu(u_b), jax.nn.gelu(v_b), v_g, w_s, b_s) * jax.nn.silu(z_b)
    y_c = gla_branch(q_c, k_c, v_c, a_r, w_a2, b_a, o_g) * jax.nn.silu(z_c)
    g = jax.nn.sigmoid(gate_logits.astype(jnp.float32)).astype(x.dtype).reshape(B, S, N_BRANCH, D)
    merged = (g[:, :, 0] * (y_a @ w_branch[0])
              + g[:, :, 1] * (y_b @ w_branch[1])
              + g[:, :, 2] * (y_c @ w_branch[2]))
    return x + merged @ w_out


def setup_inputs(seed: int = 0) -> dict:
    key = jax.random.key(seed)
    ks = jax.random.split(key, 20)
    f32 = jnp.float32

    def nrm(k, shape, fan_in, mult=1.0):
        return jax.random.normal(k, shape, f32) * (fan_in ** -0.5) * mult

    def gain(k, shape):
        return 1.0 + 0.02 * jax.random.normal(k, shape, f32)

    x = jax.random.normal(ks[0], (BATCH, SEQ, D_MODEL), f32)
    offset = jax.random.randint(ks[1], (BATCH, 1), 0, SEQ, dtype=jnp.int32)
    positions = (offset + jnp.arange(SEQ, dtype=jnp.int32)[None, :]).astype(jnp.int32)
    return {
        "x": x,
        "positions": positions,
        "norm_g": gain(ks[2], (DEPTH, D_MODEL)),
        "w_in": nrm(ks[3], (DEPTH, D_MODEL, IN_COLS), D_MODEL),
        "mla_cq_norm": gain(ks[4], (DEPTH, MLA_Q_RANK)),
        "mla_ckv_norm": gain(ks[5], (DEPTH, MLA_KV_RANK)),
        "mla_w_uq": nrm(ks[6], (DEPTH, MLA_Q_RANK, MLA_HEADS * MLA_QK), MLA_Q_RANK),
        "mla_w_ukv": nrm(ks[7], (DEPTH, MLA_KV_RANK, MLA_HEADS * (MLA_NOPE + MLA_V)), MLA_KV_RANK),
        "mla_q_norm": gain(ks[8], (DEPTH, MLA_QK)),
        "mla_k_norm": gain(ks[9], (DEPTH, MLA_QK)),
        "sgu_v_norm": gain(ks[10], (DEPTH, SGU_WIDTH)),
        "sgu_w_s": nrm(ks[11], (DEPTH, SGU_GROUPS, SGU_CHUNK, SGU_CHUNK), SGU_CHUNK),
        "sgu_b_s": 1.0 + 0.02 * jax.random.normal(ks[12], (DEPTH, SGU_GROUPS, SGU_CHUNK), f32),
        "gla_w_a2": nrm(ks[13], (DEPTH, GLA_GATE_RANK, GLA_HEADS * GLA_DK), GLA_GATE_RANK),
        "gla_b_a": 0.1 * jax.random.normal(ks[14], (DEPTH, GLA_HEADS * GLA_DK), f32),
        "gla_o_norm": gain(ks[15], (DEPTH, GLA_DV)),
        "w_branch": nrm(ks[16], (DEPTH, N_BRANCH, BRANCH_WIDTH, D_MODEL), BRANCH_WIDTH),
        "w_out": nrm(ks[17], (DEPTH, D_MODEL, D_MODEL), D_MODEL, 0.5),
    }


def reference(x, positions, norm_g, w_in, mla_cq_norm, mla_ckv_norm, mla_w_uq, mla_w_ukv,
              mla_q_norm, mla_k_norm, sgu_v_norm, sgu_w_s, sgu_b_s, gla_w_a2, gla_b_a,
              gla_o_norm, w_branch, w_out):
    cos, sin = rope_tables(positions, x.dtype)
    for l in range(DEPTH):
        x = hybrid_layer(x, cos, sin, norm_g[l], w_in[l], mla_cq_norm[l], mla_ckv_norm[l],
                         mla_w_uq[l], mla_w_ukv[l], mla_q_norm[l], mla_k_norm[l],
                         sgu_v_norm[l], sgu_w_s[l], sgu_b_s[l], gla_w_a2[l], gla_b_a[l],
                         gla_o_norm[l], w_branch[l], w_out[l])
    return x
```

```python
import os
import numpy as np
import ml_dtypes
from contextlib import ExitStack
import concourse.bass as bass
import concourse.mybir as mybir
from concourse.bass_utils import run_bass_kernel_spmd

F32 = mybir.dt.float32
BF16 = mybir.dt.bfloat16
I32 = mybir.dt.int32
AF = mybir.ActivationFunctionType
ALU = mybir.AluOpType

D = 2048
DEPTH = 2
NH = 8
EPS = 1e-6
IN_COLS = 14416
TWO_PI = 6.283185
GCUT = int(os.environ.get('GCUT', '99'))
HS_ENG = os.environ.get('HS_ENG', 'dve')

ENGS = ['pe', 'act', 'dve', 'pool', 'sp']
EPOCH = 24000
NDMA = 12


class Prog:
    def __init__(self, nc, stack):
        self.nc = nc
        self.stack = stack
        self.lists = {e: [] for e in ENGS}
        self.cnt = {e: 0 for e in ENGS}
        self.cursem = {}
        self.allsems = []
        for e in ENGS:
            self.cursem[e] = self._newsem(f"c_{e}")
        self.known = {e: {} for e in ENGS}
        self.lastw = {}
        self.readers = {}
        self.dmasems = {}
        self.dmacnt = {}
        self.dmaidx = {}
        for q in ('sp', 'act', 'pool'):
            self.dmasems[q] = [self._newsem(f"d_{q}{i}") for i in range(NDMA)]
            self.dmacnt[q] = [0] * NDMA
            self.dmaidx[q] = 0
        self.outstanding = {}
        self.enabled = True

    def _newsem(self, name):
        s = self.stack.enter_context(self.nc.semaphore(name + f"_{len(self.allsems)}"))
        self.allsems.append(s)
        return s

    def _need(self, eng, reads, writes, extra=()):
        need = {}

        def add(tok):
            if tok is None:
                return
            s, v = tok
            if need.get(s, 0) < v:
                need[s] = v
        for r in reads:
            add(self.lastw.get(r))
        for w in writes:
            add(self.lastw.get(w))
            for s, v in self.readers.get(w, {}).items():
                add((s, v))
        for t in extra:
            add(t)
        waits = []
        kn = self.known[eng]
        for s, v in need.items():
            if eng == 'pe' and s is self.cursem['pe']:
                continue
            if kn.get(s, 0) >= v:
                continue
            kn[s] = v
            waits.append((s, v))
        return waits

    def _commit(self, tok, reads, writes):
        s, v = tok
        for r in reads:
            d = self.readers.setdefault(r, {})
            if d.get(s, 0) < v:
                d[s] = v
        for w in writes:
            self.lastw[w] = tok
            self.readers[w] = {}
        self.outstanding[s] = v

    def op(self, eng, fn, reads=(), writes=()):
        if not self.enabled:
            return None
        waits = self._need(eng, reads, writes)
        if self.cnt[eng] >= EPOCH:
            self.cursem[eng] = self._newsem(f"c_{eng}")
            self.cnt[eng] = 0
        self.cnt[eng] += 1
        tok = (self.cursem[eng], self.cnt[eng])
        self.lists[eng].append((waits, fn, (tok[0], 1)))
        self._commit(tok, reads, writes)
        return tok

    def dma(self, q, out, in_, reads=(), writes=()):
        if not self.enabled:
            return None
        i = self.dmaidx[q]
        self.dmaidx[q] = (i + 1) % NDMA
        sem = self.dmasems[q][i]
        prev = self.dmacnt[q][i]
        extra = [(sem, prev)] if prev > 0 else []
        waits = self._need(q, reads, writes, extra)
        if prev + 16 > EPOCH:
            sem = self._newsem(f"d_{q}{i}")
            self.dmasems[q][i] = sem
            prev = 0
        self.dmacnt[q][i] = prev + 16
        tok = (sem, prev + 16)

        def fn(e, out=out, in_=in_):
            return e.dma_start(out=out, in_=in_)
        self.lists[q].append((waits, fn, (sem, 16)))
        self._commit(tok, reads, writes)
        return tok

    def barrier(self):
        for e in ENGS:
            kn = self.known[e]
            waits = []
            for s, v in self.outstanding.items():
                if kn.get(s, 0) >= v:
                    continue
                kn[s] = v
                waits.append((s, v))
            if waits:
                self.lists[e].append((waits, None, None))
        self.lastw = {}
        self.readers = {}

    def emit(self):
        nc = self.nc
        engmap = {'pe': 'tensor', 'act': 'scalar', 'dve': 'vector', 'pool': 'gpsimd', 'sp': 'sync'}
        with nc.Block() as block:
            for e in ENGS:
                lst = self.lists[e]

                def body(eng, lst=lst):
                    for waits, fn, inc in lst:
                        for s, v in waits:
                            eng.wait_ge(s, v)
                        if fn is not None:
                            ins = fn(eng)
                            ins.then_inc(inc[0], inc[1])
                getattr(block, engmap[e])(body)


def win_chunks():
    ch = []
    def fm(name, c0, n, cw=128):
        for i in range(n // cw):
            ch.append((name, i, np.arange(c0 + i * cw, c0 + (i + 1) * cw), cw, 'fm'))
    def tm(name, c0, n):
        for i in range(n // 512):
            ch.append((name, i, np.arange(c0 + i * 512, c0 + (i + 1) * 512), 512, 'tm'))
    fm('cq', 0, 512)
    fm('ckv', 512, 512)
    ch.append(('krA', 0, np.arange(1024, 1088), 64, 'fm'))
    ch.append(('krB', 0, np.concatenate([np.arange(1056, 1088), np.arange(1024, 1056)]), 64, 'fm'))
    ch.append(('ar', 0, np.arange(7232, 7248), 16, 'fm'))
    tm('kctok', 5696, 512)
    fm('qc', 5184, 512)
    fm('kc', 5696, 512)
    ch.append(('vc', 0, np.arange(6208, 6720), 512, 'tm'))
    fm('za', 1088, 1024)
    ch.append(('vc', 1, np.arange(6720, 7232), 512, 'tm'))
    fm('zb', 4160, 1024)
    ch.append(('vb', 0, np.arange(3136, 3648), 512, 'tm'))
    fm('zc', 7248, 1024)
    ch.append(('vb', 1, np.arange(3648, 4160), 512, 'tm'))
    fm('ub', 2112, 1024)
    fm('g', 8272, 6144)
    return ch


CHUNKS = win_chunks()
WIN_R_COLS = 16 * sum(c[3] for c in CHUNKS)

V_GT, V_CQG, V_CKVG, V_GQN, V_GQA, V_GQB, V_GKN, V_GKA, V_GKB, V_OG, NVEC = 0, 16, 20, 24, 25, 26, 27, 28, 29, 30, 32


def build(S, depth, upto=99, skip=()):
    NT = S // 128
    NB = S // 512
    nc = bass.Bass("TRN2", target_bir_lowering=False)
    dram_in = lambda name, shape, dt: nc.dram_tensor(name, shape, dt, kind="ExternalInput").ap()
    dram_sc = lambda name, shape, dt: nc.dram_tensor(name, shape, dt).ap()
    x_in = dram_in("x", [S, D], F32)
    posr = dram_in("posr", [64, S], I32)
    cf_d = dram_in("cf", [128, 128 * 3 + 512 + 4], F32)
    cb_d = dram_in("cb", [128, 384], BF16)
    W = []
    for l in range(depth):
        W.append(dict(
            win=dram_in(f"win{l}", [128, WIN_R_COLS], F32),
            wuq=dram_in(f"wuq{l}", [128, NH * 1024], F32),
            wukv=dram_in(f"wukv{l}", [128, NH * 1024], F32),
            vecs=dram_in(f"vecs{l}", [128, NVEC], F32),
            vgb=dram_in(f"vgb{l}", [128, 1024], F32),
            wsT=dram_in(f"wsT{l}", [128, 1024], F32),
            bs=dram_in(f"bs{l}", [1, 1024], F32),
            wa2=dram_in(f"wa2{l}", [16, 512], F32),
            ba=dram_in(f"ba{l}", [1, 512], F32),
            wbr=dram_in(f"wbr{l}", [128, 3 * 8 * D], F32),
            wout=dram_in(f"wout{l}", [128, 16 * D], F32),
        ))
    out_d = nc.dram_tensor("out", [S, D], F32, kind="ExternalOutput").ap()
    xmid = dram_sc("xmid", [S, D], F32)
    sc = dict(
        cq=dram_sc("s_cq", [512, S], BF16), ckv=dram_sc("s_ckv", [512, S], BF16),
        krA=dram_sc("s_krA", [64, S], BF16), krB=dram_sc("s_krB", [64, S], BF16),
        ar=dram_sc("s_ar", [16, S], BF16),
        qc=dram_sc("s_qc", [512, S], BF16), kc=dram_sc("s_kc", [512, S], BF16),
        kctok=dram_sc("s_kctok", [S, 512], BF16), vc=dram_sc("s_vc", [S, 1024], BF16),
        za=dram_sc("s_za", [1024, S], BF16), zb=dram_sc("s_zb", [1024, S], BF16), zc=dram_sc("s_zc", [1024, S], BF16),
        ub=dram_sc("s_ub", [1024, S], BF16), vb=dram_sc("s_vb", [S, 1024], BF16),
        g=dram_sc("s_g", [6144, S], BF16),
        ya=dram_sc("s_ya", [1024, S], BF16), yb=dram_sc("s_yb", [1024, S], BF16), yc=dram_sc("s_yc", [1024, S], BF16),
        mT=dram_sc("s_mT", [D, S], BF16),
    )

    with ExitStack() as st:
        P = Prog(nc, st)

        def MM(out, lhsT, rhs, s, e, r, w):
            P.op('pe', lambda en: en.matmul(out, lhsT, rhs, start=s, stop=e), r, w)

        def ACT(out, in_, func, r, w, **kw):
            P.op('act', lambda en: en.activation(out, in_, func, **kw), r, w)

        def TT(eng, out, a, b, op, r, w):
            P.op(eng, lambda en: en.tensor_tensor(out, a, b, op), r, w)

        def TS(eng, out, a, s1, s2, op0, op1, r, w):
            if s2 is None:
                P.op(eng, lambda en: en.tensor_scalar(out, a, s1, None, op0), r, w)
            else:
                P.op(eng, lambda en: en.tensor_scalar(out, a, s1, s2, op0, op1), r, w)

        def STT(eng, out, a, s, b, op0, op1, r, w):
            P.op(eng, lambda en: en.scalar_tensor_tensor(out, a, s, b, op0, op1), r, w)

        def CP(eng, out, a, r, w):
            if eng == 'act':
                P.op(eng, lambda en: en.activation(out, a, AF.Copy), r, w)
            else:
                P.op(eng, lambda en: en.tensor_copy(out, a), r, w)

        def MEMSET(eng, out, val, w):
            P.op(eng, lambda en: en.memset(out, val), (), w)

        def RECIP(out, a, r, w):
            P.op('dve', lambda en: en.reciprocal(out, a), r, w)

        def rstd_from(out, ssum, n, r, w, tmp, tmpk):
            ACT(tmp, ssum, AF.Ln, list(r), [tmpk], scale=1.0 / n, bias=EPS)
            ACT(out, tmp, AF.Exp, [tmpk], list(w), scale=-0.5)

        uid = [0]

        class Scope:
            def __init__(self):
                self.es = ExitStack()
            def __enter__(self):
                self.es.__enter__()
                return self
            def __exit__(self, *a):
                P.barrier()
                return self.es.__exit__(*a)
            def T(self, name, shape, dt):
                uid[0] += 1
                return self.es.enter_context(nc.sbuf_tensor(f"t{uid[0]}_{name}", shape, dt))

        gs = Scope()
        gs.__enter__()
        cf = gs.T("cf", [128, 128 * 3 + 512 + 4], F32)
        cb = gs.T("cbt", [128, 384], BF16)
        P.dma('sp', cf[:], cf_d, (), ['cf'])
        P.dma('sp', cb[:], cb_d, (), ['cb'])
        U_f = cf[:, 0:128]
        Urev_f = cf[:, 128:256]
        ones_f = cf[:, 256:384]
        U4_f = cf[:, 384:896]
        invf2 = cf[0:64, 896:897]
        sgn = cf[0:64, 897:898]
        ident_b = cb[:, 0:128]
        U_b = cb[:, 128:256]
        ones_b = cb[:, 256:384]
        ps = [st.enter_context(nc.psum_tensor(f"ps{i}", [128, 512], F32)) for i in range(8)]
        ptv = [ps[6][:].bitcast(BF16), ps[7][:].bitcast(BF16)]
        PK = [f"ps{i}" for i in range(8)]

        def rope_tables(cos2, sin2):
            s0 = Scope()
            s0.__enter__()
            pi_t = s0.T("pos_i", [64, S], I32)
            u_t = s0.T("rp_u", [64, S], F32)
            r_i = s0.T("rp_ri", [64, S], I32)
            r_f = s0.T("rp_rf", [64, S], F32)
            m_t = s0.T("rp_m", [64, S], F32)
            P.dma('sp', pi_t[:], posr, (), ['pos_i'])
            CP('dve', u_t[:], pi_t[:], ['pos_i'], ['u'])
            TS('dve', u_t[:], u_t[:], invf2, 1.0 / (2 * np.pi), ALU.mult, ALU.mult, ['u', 'cf'], ['u'])
            for which, dst in (('sin', sin2), ('cos', cos2)):
                if which == 'cos':
                    TS('dve', u_t[:], u_t[:], 0.25, None, ALU.add, None, ['u'], ['u'])
                CP('dve', r_i[:], u_t[:], ['u'], ['ri'])
                CP('dve', r_f[:], r_i[:], ['ri'], ['rf'])
                TT('dve', r_f[:], u_t[:], r_f[:], ALU.subtract, ['u', 'rf'], ['rf'])
                TS('dve', m_t[:], r_f[:], 0.5, None, ALU.is_gt, None, ['rf'], ['m'])
                TT('dve', r_f[:], r_f[:], m_t[:], ALU.subtract, ['rf', 'm'], ['rf'])
                TS('dve', m_t[:], r_f[:], -0.5, None, ALU.is_lt, None, ['rf'], ['m'])
                TT('dve', r_f[:], r_f[:], m_t[:], ALU.add, ['rf', 'm'], ['rf'])
                ACT(dst[:], r_f[:], AF.Sin, ['rf'], [which], scale=TWO_PI)
            TS('dve', sin2[:], sin2[:], sgn, None, ALU.mult, None, ['sin', 'cf'], ['sin'])
            en_ = P.enabled
            P.enabled = True
            s0.__exit__(None, None, None)
            P.enabled = en_

        for l in range(depth):
            Wl = W[l]
            x_src = x_in if l == 0 else xmid
            x_dst = out_d if l == depth - 1 else xmid
            ls = Scope()
            ls.__enter__()
            vecs = ls.T("vecs", [128, NVEC], F32)
            P.dma('sp', vecs[:], Wl['vecs'], (), ['vecs'])

            with Scope() as s12:
                hT = s12.T("hT", [128, 16, S], BF16)
                with Scope() as s1:
                    P.enabled = (upto >= 1) and (1 not in skip)
                    xt = [s1.T(f"xt{i}", [128, D], F32) for i in range(2)]
                    hs = [s1.T(f"hs{i}", [128, D], BF16) for i in range(2)]
                    junk = s1.T("junk", [128, D], BF16)
                    ss = [s1.T(f"ss{i}", [128, 1], F32) for i in range(2)]
                    sq = [s1.T(f"sq{i}", [128, 1], F32) for i in range(2)]
                    rs = [s1.T(f"rs{i}", [128, 1], F32) for i in range(2)]
                    G = s1.T("G", [128, 16, 128], F32)
                    for kc in range(16):
                        TS('dve', G[:, kc, :], ones_f, vecs[:, V_GT + kc:V_GT + kc + 1], None, ALU.mult, None, ['cf', 'vecs'], ['G'])
                    P.dma('sp', xt[0][:], x_src[0:128, :], [('xd', 0)], ['xt0'])
                    for tt in range(NT):
                        b = tt % 2
                        if tt + 1 < NT:
                            P.dma('sp', xt[(tt + 1) % 2][:], x_src[(tt + 1) * 128:(tt + 2) * 128, :], [('xd', tt + 1)], [f'xt{(tt + 1) % 2}'])
                        MEMSET('pool', ss[b][:], 0.0, [f'ss{b}'])
                        ACT(junk[:], xt[b][:], AF.Square, [f'xt{b}', f'ss{b}'], ['junk', f'ss{b}'], accum_out=ss[b][:])
                        rstd_from(rs[b][:], ss[b][:], D, [f'ss{b}'], [f'rs{b}'], sq[b][:], f'sq{b}')
                        TS('dve', hs[b][:, 0:1024], xt[b][:, 0:1024], rs[b][:, 0:1], None, ALU.mult, None, [f'xt{b}', f'rs{b}'], [f'hs{b}a'])
                        TS(HS_ENG, hs[b][:, 1024:2048], xt[b][:, 1024:2048], rs[b][:, 0:1], None, ALU.mult, None, [f'xt{b}', f'rs{b}'], [f'hs{b}b'])
                        for kq in range(4):
                            half = (tt * 4 + kq) % 2
                            for j in range(4):
                                kc = kq * 4 + j
                                P.op('pe', (lambda o, i: (lambda en: en.transpose(o, i, ident_b)))(
                                    ptv[half][:, j * 128:(j + 1) * 128], hs[b][:, kc * 128:(kc + 1) * 128]),
                                    [f'hs{b}a' if kq < 2 else f'hs{b}b', 'cb'], [PK[6 + half]])
                            TT('dve', hT[:, kq * 4:(kq + 1) * 4, tt * 128:(tt + 1) * 128], ptv[half][:, 0:512].rearrange("p (j t) -> p j t", t=128),
                               G[:, kq * 4:(kq + 1) * 4, :], ALU.mult, [PK[6 + half], 'G'], [('hT', tt)])
                with Scope() as s2:
                    P.enabled = (upto >= 2) and (2 not in skip)
                    wf = [s2.T(f"wf{i}", [128, 16, 128], BF16) for i in range(3)]
                    wt = [s2.T(f"wt{i}", [128, 16, 512], BF16) for i in range(1)]
                    ob = [s2.T(f"ob{i}", [128, 512], BF16) for i in range(4)]
                    nfm = 0
                    ntm = 0
                    nps = 0
                    nob = 0
                    off = 0
                    for (name, idx, cols, cw, mode) in CHUNKS:
                        src = Wl['win'][:, 16 * off:16 * (off + cw)].rearrange("p (k c) -> p k c", c=cw)
                        off += cw
                        if mode == 'fm':
                            wb = nfm % 3
                            nfm += 1
                            P.dma('pool', wf[wb][:, :, 0:cw], src, (), [f'wf{wb}'])
                            func = {'za': AF.Silu, 'zb': AF.Silu, 'zc': AF.Silu, 'ub': AF.Gelu_apprx_tanh, 'g': AF.Sigmoid}.get(name)
                            for tb in range(NB):
                                pb = nps % 6
                                nps += 1
                                for kc in range(16):
                                    MM(ps[pb][0:cw, :], wf[wb][:, kc, 0:cw], hT[:, kc, tb * 512:(tb + 1) * 512], kc == 0, kc == 15,
                                       [f'wf{wb}'] + [('hT', tb * 4 + i) for i in range(4)], [PK[pb]])
                                o = nob % 4
                                nob += 1
                                if func is not None:
                                    ACT(ob[o][0:cw, :], ps[pb][0:cw, :], func, [PK[pb]], [f'ob{o}'])
                                else:
                                    CP('dve', ob[o][0:cw, :], ps[pb][0:cw, :], [PK[pb]], [f'ob{o}'])
                                P.dma('sp', sc[name][idx * cw:(idx + 1) * cw, tb * 512:(tb + 1) * 512], ob[o][0:cw, :], [f'ob{o}'], [(name, idx, tb)])
                        else:
                            wb = 0
                            ntm += 1
                            P.dma('pool', wt[wb][:], src, (), [f'wt{wb}'])
                            func = AF.Gelu_apprx_tanh if name == 'vb' else None
                            for tt in range(NT):
                                pb = nps % 6
                                nps += 1
                                for kc in range(16):
                                    MM(ps[pb][:], hT[:, kc, tt * 128:(tt + 1) * 128], wt[wb][:, kc, :], kc == 0, kc == 15,
                                       [f'wt{wb}', ('hT', tt)], [PK[pb]])
                                o = nob % 4
                                nob += 1
                                if func is not None:
                                    ACT(ob[o][:], ps[pb][:], func, [PK[pb]], [f'ob{o}'])
                                else:
                                    CP('dve', ob[o][:], ps[pb][:], [PK[pb]], [f'ob{o}'])
                                P.dma('sp', sc[name][tt * 128:(tt + 1) * 128, idx * 512:(idx + 1) * 512], ob[o][:], [f'ob{o}'], [(name, idx, tt)])

            with Scope() as s3:
                P.enabled = (upto >= 3) and (3 not in skip)
                cos2 = s3.T("cos2", [64, S], F32)
                sin2 = s3.T("sin2", [64, S], F32)
                rope_tables(cos2, sin2)
                cqn = s3.T("cqn", [128, 4, S], BF16)
                ckvn = s3.T("ckvn", [128, 4, S], BF16)
                krA = s3.T("krA", [64, S], BF16)
                krB = s3.T("krB", [64, S], BF16)
                P.dma('sp', krA[:], sc['krA'], (), ['krA'])
                P.dma('sp', krB[:], sc['krB'], (), ['krB'])
                tmpa = [s3.T(f"tmpa{i}", [128, 512], F32) for i in range(2)]
                rst = [s3.T(f"rst{i}", [128, 512], F32) for i in range(2)]
                s3p = Scope()
                s3p.__enter__()
                sqt = [s3p.T(f"sqt{i}", [128, 4, 512], BF16) for i in range(2)]
                cin = [s3p.T(f"cin{i}", [128, 4, 512], BF16) for i in range(2)]
                n = 0
                for (nm, dst, gcol) in (('cq', cqn, V_CQG), ('ckv', ckvn, V_CKVG)):
                    srcv = sc[nm].rearrange("(k p) s -> p k s", p=128)
                    for tb in range(NB):
                        b = n % 2
                        n += 1
                        P.dma('sp', cin[b][:], srcv[:, :, tb * 512:(tb + 1) * 512], (), [f'cin{b}'])
                        ACT(sqt[b][:], cin[b][:], AF.Square, [f'cin{b}'], [f'sqt{b}'])
                        for kc in range(4):
                            MM(ps[b][:], ones_b, sqt[b][:, kc, :], kc == 0, kc == 3, ['cb', f'sqt{b}'], [PK[b]])
                        rstd_from(rst[b][:], ps[b][:], 512, [PK[b]], [f'rst{b}'], tmpa[b][:], f'tmpa{b}')
                        for kc in range(4):
                            STT('dve', dst[:, kc, tb * 512:(tb + 1) * 512], cin[b][:, kc, :], vecs[:, gcol + kc:gcol + kc + 1], rst[b][:],
                                ALU.mult, ALU.mult, [f'cin{b}', 'vecs', f'rst{b}'], [(nm + 'n', tb)])
                en_ = P.enabled
                P.enabled = True
                s3p.__exit__(None, None, None)
                P.enabled = en_
                Qn = s3.T("Qn", [128, S], BF16)
                Qr = s3.T("Qr", [64, S], BF16)
                Kn = s3.T("Kn", [128, S], BF16)
                Kr = s3.T("Kr", [64, S], BF16)
                Vh = s3.T("Vh", [128, NT, 128], BF16)
                wq = s3.T("wq", [128, 1024], BF16)
                wkv = s3.T("wkv", [128, 1024], BF16)
                sqn = [s3.T(f"sqn{i}", [128, 512], BF16) for i in range(2)]
                sqa = [s3.T(f"sqa{i}", [64, 512], BF16) for i in range(2)]
                An = [s3.T(f"An{i}", [64, 512], F32) for i in range(2)]
                Bn = [s3.T(f"Bn{i}", [64, 512], F32) for i in range(2)]
                PT = [s3.T(f"PT{i}", [128, 512], BF16) for i in range(4)]
                zat = [s3.T(f"zat{i}", [128, 512], BF16) for i in range(2)]
                rL = [s3.T(f"rL{i}", [128, 512], F32) for i in range(2)]
                yf = [s3.T(f"yf{i}", [128, 512], F32) for i in range(2)]
                yo = [s3.T(f"yo{i}", [128, 512], BF16) for i in range(2)]
                SCALE = 192 ** -0.5
                it = 0
                for h in range(NH):
                    P.dma('pool', wq[:], Wl['wuq'][:, h * 1024:(h + 1) * 1024], (), ['wq'])
                    P.dma('pool', wkv[:], Wl['wukv'][:, h * 1024:(h + 1) * 1024], (), ['wkv'])
                    wq_n = lambda kc: wq[:, kc * 128:(kc + 1) * 128]
                    wq_a = lambda kc: wq[:, 512 + kc * 64:512 + (kc + 1) * 64]
                    wq_b = lambda kc: wq[:, 768 + kc * 64:768 + (kc + 1) * 64]
                    wk_n = lambda kc: wkv[:, kc * 128:(kc + 1) * 128]
                    wv = lambda kc: wkv[:, 512 + kc * 128:512 + (kc + 1) * 128]
                    for tb in range(NB):
                        sl = slice(tb * 512, (tb + 1) * 512)
                        bq = it % 2
                        bk = (it + 1) % 2
                        it += 2
                        for kc in range(4):
                            MM(ps[0][:], wq_n(kc), cqn[:, kc, sl], kc == 0, kc == 3, ['wq', ('cqn', tb)], [PK[0]])
                        for kc in range(4):
                            MM(ps[1][0:64, :], wq_a(kc), cqn[:, kc, sl], kc == 0, kc == 3, ['wq', ('cqn', tb)], [PK[1]])
                        for kc in range(4):
                            MM(ps[2][0:64, :], wq_b(kc), cqn[:, kc, sl], kc == 0, kc == 3, ['wq', ('cqn', tb)], [PK[2]])
                        for kc in range(4):
                            MM(ps[4][:], wk_n(kc), ckvn[:, kc, sl], kc == 0, kc == 3, ['wkv', ('ckvn', tb)], [PK[4]])
                        for j in range(4):
                            tt = tb * 4 + j
                            for kc in range(4):
                                MM(ps[6][:, j * 128:(j + 1) * 128], ckvn[:, kc, tt * 128:(tt + 1) * 128], wv(kc), kc == 0, kc == 3,
                                   ['wkv', ('ckvn', tb)], [PK[6]])
                        b = bq
                        ACT(sqn[b][:], ps[0][:], AF.Square, [PK[0]], [f'sqn{b}'])
                        ACT(sqa[b][:], ps[1][0:64, :], AF.Square, [PK[1]], [f'sqa{b}'])
                        b = bk
                        ACT(sqn[b][:], ps[4][:], AF.Square, [PK[4]], [f'sqn{b}'])
                        ACT(sqa[b][:], krA[:, sl], AF.Square, ['krA'], [f'sqa{b}'])
                        CP('act', Vh[:, tb * 4:(tb + 1) * 4, :], ps[6][:].rearrange("p (j d) -> p j d", d=128), [PK[6]], [('Vh', tb)])
                        b = bq
                        MM(ps[3][:], ones_b, sqn[b][:], True, False, ['cb', f'sqn{b}'], [PK[3]])
                        MM(ps[3][:], cb[0:64, 256:384], sqa[b][:], False, True, ['cb', f'sqa{b}'], [PK[3]])
                        b = bk
                        MM(ps[5][:], ones_b, sqn[b][:], True, False, ['cb', f'sqn{b}'], [PK[5]])
                        MM(ps[5][:], cb[0:64, 256:384], sqa[b][:], False, True, ['cb', f'sqa{b}'], [PK[5]])
                        b = bq
                        rstd_from(rst[b][:], ps[3][:], 192, [PK[3]], [f'rst{b}'], tmpa[b][:], f'tmpa{b}')
                        STT('dve', Qn[:, sl], ps[0][:], vecs[:, V_GQN:V_GQN + 1], rst[b][:], ALU.mult, ALU.mult, [PK[0], 'vecs', f'rst{b}'], [('Qn', tb)])
                        STT('dve', An[b][:], ps[1][0:64, :], vecs[0:64, V_GQA:V_GQA + 1], rst[b][0:64, :], ALU.mult, ALU.mult, [PK[1], 'vecs', f'rst{b}'], [f'An{b}'])
                        STT('dve', Bn[b][:], ps[2][0:64, :], vecs[0:64, V_GQB:V_GQB + 1], rst[b][0:64, :], ALU.mult, ALU.mult, [PK[2], 'vecs', f'rst{b}'], [f'Bn{b}'])
                        TT('pool', An[b][:], An[b][:], cos2[:, sl], ALU.mult, [f'An{b}', 'cos'], [f'An{b}'])
                        TT('pool', Bn[b][:], Bn[b][:], sin2[:, sl], ALU.mult, [f'Bn{b}', 'sin'], [f'Bn{b}'])
                        TT('pool', Qr[:, sl], An[b][:], Bn[b][:], ALU.add, [f'An{b}', f'Bn{b}'], [('Qr', tb)])
                        b = bk
                        rstd_from(rst[b][:], ps[5][:], 192, [PK[5]], [f'rst{b}'], tmpa[b][:], f'tmpa{b}')
                        STT('dve', Kn[:, sl], ps[4][:], vecs[:, V_GKN:V_GKN + 1], rst[b][:], ALU.mult, ALU.mult, [PK[4], 'vecs', f'rst{b}'], [('Kn', tb)])
                        STT('dve', An[b][:], krA[:, sl], vecs[0:64, V_GKA:V_GKA + 1], rst[b][0:64, :], ALU.mult, ALU.mult, ['krA', 'vecs', f'rst{b}'], [f'An{b}'])
                        STT('dve', Bn[b][:], krB[:, sl], vecs[0:64, V_GKB:V_GKB + 1], rst[b][0:64, :], ALU.mult, ALU.mult, ['krB', 'vecs', f'rst{b}'], [f'Bn{b}'])
                        TT('pool', An[b][:], An[b][:], cos2[:, sl], ALU.mult, [f'An{b}', 'cos'], [f'An{b}'])
                        TT('pool', Bn[b][:], Bn[b][:], sin2[:, sl], ALU.mult, [f'Bn{b}', 'sin'], [f'Bn{b}'])
                        TT('pool', Kr[:, sl], An[b][:], Bn[b][:], ALU.add, [f'An{b}', f'Bn{b}'], [('Kr', tb)])
                    for qb in range(NB):
                        ob_ = qb % 2
                        pO, pL = ps[3 + ob_ * 2], ps[4 + ob_ * 2]
                        kO, kL = PK[3 + ob_ * 2], PK[4 + ob_ * 2]
                        P.dma('sp', zat[ob_][:], sc['za'][h * 128:(h + 1) * 128, qb * 512:(qb + 1) * 512], (), [f'zat{ob_}'])
                        nk = 4 * qb + 4
                        SB = [0, 1, 2, 7]

                        def s_part(kt):
                            j = max(0, kt - 4 * qb)
                            c0 = j * 128
                            sb_ = kt % 4
                            pS, kS = ps[SB[sb_]], PK[SB[sb_]]
                            qs = slice(qb * 512 + c0, (qb + 1) * 512)
                            ks = slice(kt * 128, (kt + 1) * 128)
                            MM(pS[:, c0:512], Kn[:, ks], Qn[:, qs], True, False, [('Kn', kt // 4), ('Qn', qb)], [kS])
                            MM(pS[:, c0:512], Kr[:, ks], Qr[:, qs], False, True, [('Kr', kt // 4), ('Qr', qb)], [kS])
                            ACT(PT[sb_][:, c0:512], pS[:, c0:512], AF.Exp, [kS], [f'PT{sb_}'], scale=SCALE)
                            if kt >= 4 * qb:
                                TT('pool', PT[sb_][:, c0:c0 + 128], PT[sb_][:, c0:c0 + 128], U_b, ALU.mult, [f'PT{sb_}', 'cb'], [f'PT{sb_}'])

                        def pv_part(kt):
                            j = max(0, kt - 4 * qb)
                            c0 = j * 128
                            sb_ = kt % 4
                            MM(pO[:, c0:512], Vh[:, kt, :], PT[sb_][:, c0:512], kt == 0, kt == nk - 1, [('Vh', kt // 4), f'PT{sb_}'], [kO])
                            MM(pL[:, c0:512], ones_b, PT[sb_][:, c0:512], kt == 0, kt == nk - 1, ['cb', f'PT{sb_}'], [kL])
                        LOOK = 2
                        for kt in range(nk + LOOK):
                            if kt < nk:
                                s_part(kt)
                            if kt >= LOOK:
                                pv_part(kt - LOOK)
                        RECIP(rL[ob_][:], pL[:], [kL], [f'rL{ob_}'])
                        TT('dve', yf[ob_][:], pO[:], rL[ob_][:], ALU.mult, [kO, f'rL{ob_}'], [f'yf{ob_}'])
                        TT('pool', yo[ob_][:], yf[ob_][:], zat[ob_][:], ALU.mult, [f'yf{ob_}', f'zat{ob_}'], [f'yo{ob_}'])
                        P.dma('sp', sc['ya'][h * 128:(h + 1) * 128, qb * 512:(qb + 1) * 512], yo[ob_][:], [f'yo{ob_}'], [('ya', h, qb)])

            sW = Scope()
            sW.__enter__()
            wbr = sW.T("wbr", [128, 3, 8, D], BF16)
            wbv = Wl['wbr'].rearrange("p (b k n) -> p b k n", b=3, k=8)
            wbr_jobs = [(cg, bb, kc) for cg in range(4) for bb in range(3) for kc in range(8)]

            def wbr_prefetch(n_):
                en_ = P.enabled
                P.enabled = (upto >= 7) and (7 not in skip)
                for _ in range(n_):
                    if wbr_jobs:
                        cg, bb, kc = wbr_jobs.pop(0)
                        P.dma('pool', wbr[:, bb, kc, cg * 512:(cg + 1) * 512], wbv[:, bb, kc, cg * 512:(cg + 1) * 512], (), [('wbr', cg)])
                P.enabled = en_
            with Scope() as s5:
                P.enabled = (upto >= 5) and (5 not in skip)
                vn = s5.T("vn", [128, NT, 1024], BF16)
                vgb = s5.T("vgb", [128, 1024], F32)
                wsT = s5.T("wsT", [128, 8, 128], F32)
                wsTb = s5.T("wsTb", [128, 8, 128], BF16)
                bsf = s5.T("bsf", [1, 1024], F32)
                P.dma('sp', vgb[:], Wl['vgb'], (), ['vgb'])
                P.dma('sp', wsT[:], Wl['wsT'].rearrange("p (g t) -> p g t", t=128), (), ['wsT'])
                P.dma('sp', bsf[:], Wl['bs'], (), ['bsf'])
                for g in range(8):
                    TT('pool', wsTb[:, g, :], wsT[:, g, :], U_f, ALU.mult, ['wsT', 'cf'], ['wsTb'])
                vt = [s5.T(f"vt{i}", [128, 1024], BF16) for i in range(2)]
                junk5 = s5.T("junk5", [128, 1024], BF16)
                ss5 = [s5.T(f"ss5{i}", [128, 1], F32) for i in range(2)]
                sq5 = [s5.T(f"sq5{i}", [128, 1], F32) for i in range(2)]
                rs5 = [s5.T(f"rs5{i}", [128, 1], F32) for i in range(2)]
                for tt in range(NT):
                    b = tt % 2
                    P.dma('sp', vt[b][:], sc['vb'][tt * 128:(tt + 1) * 128, :], (), [f'vt{b}'])
                    MEMSET('pool', ss5[b][:], 0.0, [f'ss5{b}'])
                    ACT(junk5[:], vt[b][:], AF.Square, [f'vt{b}', f'ss5{b}'], ['junk5', f'ss5{b}'], accum_out=ss5[b][:])
                    rstd_from(rs5[b][:], ss5[b][:], 1024, [f'ss5{b}'], [f'rs5{b}'], sq5[b][:], f'sq5{b}')
                    STT('dve', vn[:, tt, :], vt[b][:], rs5[b][:, 0:1], vgb[:], ALU.mult, ALU.mult, [f'vt{b}', f'rs5{b}', 'vgb'], [('vn', tt)])
                ut = [s5.T(f"ut{i}", [128, 512], BF16) for i in range(3)]
                zt = [s5.T(f"zt{i}", [128, 512], BF16) for i in range(3)]
                y5 = [s5.T(f"y5{i}", [128, 512], F32) for i in range(2)]
                yb5 = [s5.T(f"yb5{i}", [128, 512], BF16) for i in range(2)]
                n = 0
                its5 = [(g, tb) for g in range(8) for tb in range(NB)]

                def ld5(i):
                    g, tb = its5[i]
                    b3 = i % 3
                    P.dma('sp', ut[b3][:], sc['ub'][g * 128:(g + 1) * 128, tb * 512:(tb + 1) * 512], (), [f'ut{b3}'])
                    P.dma('sp', zt[b3][:], sc['zb'][g * 128:(g + 1) * 128, tb * 512:(tb + 1) * 512], (), [f'zt{b3}'])
                ld5(0)
                for i5, (g, tb) in enumerate(its5):
                    if True:
                        b = n % 2
                        b3 = i5 % 3
                        n += 1
                        if i5 + 1 < len(its5):
                            ld5(i5 + 1)
                        wbr_prefetch(1)
                        for j in range(4):
                            tt = tb * 4 + j
                            MM(ps[b][:, j * 128:(j + 1) * 128], vn[:, tt, g * 128:(g + 1) * 128], wsTb[:, g, :], True, False, [('vn', tt), 'wsTb'], [PK[b]])
                            MM(ps[b][:, j * 128:(j + 1) * 128], cf[0:1, 256:384], bsf[0:1, g * 128:(g + 1) * 128], False, True, ['cf', 'bsf'], [PK[b]])
                        TT('dve', y5[b][:], ps[b][:], ut[b3][:], ALU.mult, [PK[b], f'ut{b3}'], [f'y5{b}'])
                        TT('pool', yb5[b][:], y5[b][:], zt[b3][:], ALU.mult, [f'y5{b}', f'zt{b3}'], [f'yb5{b}'])
                        P.dma('act', sc['yb'][g * 128:(g + 1) * 128, tb * 512:(tb + 1) * 512], yb5[b][:], [f'yb5{b}'], [('yb', g, tb)])

            with Scope() as s6:
                P.enabled = (upto >= 6) and (6 not in skip)
                wa2f = s6.T("wa2f", [16, 512], F32)
                wa2 = s6.T("wa2", [16, 512], BF16)
                baf = s6.T("baf", [1, 512], F32)
                P.dma('sp', wa2f[:], Wl['wa2'], (), ['wa2f'])
                P.dma('sp', baf[:], Wl['ba'], (), ['baf'])
                CP('dve', wa2[:], wa2f[:], ['wa2f'], ['wa2'])
                Sf = s6.T("Sf", [128, 4, 256], F32)
                Sb = s6.T("Sb", [128, 4, 256], BF16)
                MEMSET('pool', Sf[:], 0.0, [('Sf', h) for h in range(4)])
                MEMSET('pool', Sb[:], 0.0, ['Sb'])
                mk = lambda nm, shp, dt, n: [s6.T(f"{nm}{i}", shp, dt) for i in range(n)]
                qT4 = mk("qT4", [128, 4, 128], BF16, 2)
                kT4 = mk("kT4", [128, 4, 128], BF16, 2)
                ktok = mk("ktok", [128, 512], BF16, 2)
                vtok = mk("vtok", [128, 1024], BF16, 2)
                art = mk("art", [16, 128], BF16, 2)
                zc8 = mk("zc8", [128, 8, 128], BF16, 2)
                e1 = mk("e1", [128, 512], F32, 2)
                l1 = mk("l1", [128, 512], F32, 2)
                ebT = mk("ebT", [128, 512], F32, 4)
                enbT = mk("enbT", [128, 512], F32, 2)
                ed = mk("ed", [128, 512], F32, 2)
                qg = mk("qg", [128, 4, 128], BF16, 4)
                kg = mk("kg", [128, 4, 128], BF16, 2)
                kd = mk("kd", [128, 512], BF16, 4)
                aT = mk("aT", [128, 4, 128], BF16, 2)
                sq6 = mk("sq6", [128, 8, 128], F32, 2)
                tm6 = mk("tm6", [128, 512], F32, 2)
                rs6 = mk("rs6", [128, 512], F32, 2)
                yn6 = mk("yn6", [128, 8, 128], F32, 2)
                yc6 = mk("yc6", [128, 8, 128], BF16, 2)
                qcv = sc['qc'].rearrange("(h p) s -> p h s", p=128)
                kcv = sc['kc'].rearrange("(h p) s -> p h s", p=128)
                zcv = sc['zc'].rearrange("(m p) s -> p m s", p=128)
                ycv = sc['yc'].rearrange("(m p) s -> p m s", p=128)
                ok = lambda c: 0 <= c < NT

                def L_art(c):
                    if ok(c):
                        P.dma('sp', art[c % 2][:], sc['ar'][:, c * 128:(c + 1) * 128], (), [f'art{c % 2}'])

                def L_qk(c):
                    if ok(c):
                        b = c % 2
                        cs = slice(c * 128, (c + 1) * 128)
                        P.dma('sp', qT4[b][:], qcv[:, :, cs], (), [f'qT4{b}'])
                        P.dma('sp', kT4[b][:], kcv[:, :, cs], (), [f'kT4{b}'])
                        P.dma('sp', ktok[b][:], sc['kctok'][cs, :], (), [f'ktok{b}'])

                def L_v(c):
                    if ok(c):
                        P.dma('sp', vtok[c % 2][:], sc['vc'][c * 128:(c + 1) * 128, :], (), [f'vtok{c % 2}'])

                def L_z(c):
                    if ok(c):
                        P.dma('sp', zc8[c % 2][:], zcv[:, :, c * 128:(c + 1) * 128], (), [f'zc8{c % 2}'])

                def F1(c):
                    if not ok(c):
                        return
                    b = c % 2
                    MM(ps[0][:], art[b][:], wa2[:], True, False, [f'art{b}', 'wa2'], [PK[0]])
                    MM(ps[0][:], cf[0:1, 256:384], baf[:], False, True, ['cf', 'baf'], [PK[0]])
                    ACT(e1[b][:], ps[0][:], AF.Exp, [PK[0]], [f'e1{b}'], scale=-1.0)
                    ACT(l1[b][:], e1[b][:], AF.Ln, [f'e1{b}'], [f'l1{b}'], bias=1.0)

                def F2(c):
                    if not ok(c):
                        return
                    b, b4 = c % 2, c % 4
                    for h in range(4):
                        MM(ps[1][:, h * 128:(h + 1) * 128], l1[b][:, h * 128:(h + 1) * 128], U_f, True, True, [f'l1{b}', 'cf'], [PK[1]])
                    MM(ps[2][:], Urev_f, l1[b][:], True, True, [f'l1{b}', 'cf'], [PK[2]])
                    ACT(ebT[b4][:], ps[1][:], AF.Exp, [PK[1]], [f'ebT{b4}'], scale=-1.0 / 16)
                    ACT(enbT[b][:], ps[1][:], AF.Exp, [PK[1]], [f'enbT{b}'], scale=1.0 / 16)
                    ACT(ed[b][:], ps[2][:], AF.Exp, [PK[2]], [f'ed{b}'], scale=-1.0 / 16)
                    STT('dve', qg[b4][:], qT4[b][:], 128 ** -0.5, ebT[b4][:].rearrange("p (h t) -> p h t", t=128), ALU.mult, ALU.mult,
                        [f'qT4{b}', f'ebT{b4}'], [f'qg{b4}'])
                    TT('pool', kg[b][:], kT4[b][:], enbT[b][:].rearrange("p (h t) -> p h t", t=128), ALU.mult, [f'kT4{b}', f'enbT{b}'], [f'kg{b}'])
                    TT('pool', kd[b4][:], ktok[b][:], ed[b][:], ALU.mult, [f'ktok{b}', f'ed{b}'], [f'kd{b4}'])

                def F3(c):
                    if not ok(c):
                        return
                    b, b4 = c % 2, c % 4
                    for h in range(4):
                        MM(ps[3][:, h * 128:(h + 1) * 128], kg[b][:, h, :], qg[b4][:, h, :], True, True, [f'kg{b}', f'qg{b4}'], [PK[3]])
                    TT('dve', aT[b][:], ps[3][:].rearrange("p (h t) -> p h t", t=128), U4_f.rearrange("p (h t) -> p h t", t=128), ALU.mult,
                       [PK[3], 'cf'], [f'aT{b}'])

                def B1(c):
                    if not ok(c):
                        return
                    b, b4 = c % 2, c % 4
                    for jj in range(2):
                        for h in range(4):
                            o_ = ps[4 + jj][:, h * 128:(h + 1) * 128]
                            MM(o_, vtok[b][:, h * 256 + jj * 128:h * 256 + (jj + 1) * 128], aT[b][:, h, :], True, False, [f'vtok{b}', f'aT{b}'], [PK[4 + jj]])
                            MM(o_, Sb[:, h, jj * 128:(jj + 1) * 128], qg[b4][:, h, :], False, True, ['Sb', f'qg{b4}'], [PK[4 + jj]])
                    for h in range(4):
                        pS = ps[6 + h % 2]
                        MM(pS[:, 0:256], kd[b4][:, h * 128:(h + 1) * 128], vtok[b][:, h * 256:(h + 1) * 256], True, True, [f'kd{b4}', f'vtok{b}'], [PK[6 + h % 2]])
                        TS('dve', Sf[:, h, :], Sf[:, h, :], ebT[b4][:, h * 128 + 127:h * 128 + 128], None, ALU.mult, None, [('Sf', h), f'ebT{b4}'], [('Sf', h)])
                        TT('dve', Sf[:, h, :], pS[:, 0:256], Sf[:, h, :], ALU.add, [('Sf', h), PK[6 + h % 2]], [('Sf', h)])
                    CP('act', Sb[:], Sf[:], [('Sf', h) for h in range(4)], ['Sb'])
                    for jj in range(2):
                        ACT(sq6[b][:].rearrange("p (h j) t -> p h j t", j=2)[:, :, jj, :], ps[4 + jj][:].rearrange("p (h t) -> p h t", t=128), AF.Square,
                            [PK[4 + jj]], [f'sq6{b}'])

                def B2(c):
                    if not ok(c):
                        return
                    b = c % 2
                    for h in range(4):
                        MM(ps[0][:, h * 128:(h + 1) * 128], ones_f, sq6[b][:, 2 * h, :], True, False, ['cf', f'sq6{b}'], [PK[0]])
                        MM(ps[0][:, h * 128:(h + 1) * 128], ones_f, sq6[b][:, 2 * h + 1, :], False, True, ['cf', f'sq6{b}'], [PK[0]])
                    rstd_from(rs6[b][:], ps[0][:], 256, [PK[0]], [f'rs6{b}'], tm6[b][:], f'tm6{b}')
                    for jj in range(2):
                        STT('dve', yn6[b][:].rearrange("p (h j) t -> p h j t", j=2)[:, :, jj, :], ps[4 + jj][:].rearrange("p (h t) -> p h t", t=128),
                            vecs[:, V_OG + jj:V_OG + jj + 1], rs6[b][:].rearrange("p (h t) -> p h t", t=128), ALU.mult, ALU.mult,
                            [PK[4 + jj], 'vecs', f'rs6{b}'], [f'yn6{b}'])
                    TT('pool', yc6[b][:], yn6[b][:], zc8[b][:], ALU.mult, [f'yn6{b}', f'zc8{b}'], [f'yc6{b}'])
                    P.dma('sp', ycv[:, :, c * 128:(c + 1) * 128], yc6[b][:], [f'yc6{b}'], [('yc', c)])

                for i in range(-4, NT + 1):
                    wbr_prefetch(-(-len(wbr_jobs) // max(1, NT + 1 - i)))
                    L_art(i + 4)
                    L_qk(i + 3)
                    L_v(i + 1)
                    L_z(i)
                    F1(i + 3)
                    F2(i + 2)
                    F3(i + 1)
                    B2(i - 1)
                    B1(i)

            with Scope() as s7:
                P.enabled = (upto >= 7) and (7 not in skip)
                yT = [[s7.T(f"yT{i}_{bb}", [128, 8, 512], BF16) for bb in range(3)] for i in range(2)]
                g3 = [s7.T(f"g3{i}", [128, 3, 512], BF16) for i in range(3)]
                mm_ = [[s7.T(f"mm{i}_{bb}", [128, 512], F32) for bb in range(3)] for i in range(2)]
                mo = [s7.T(f"mo{i}", [128, 512], BF16) for i in range(2)]
                gv = sc['g'].rearrange("(b c p) s -> p b c s", b=3, p=128)
                n = 0

                def ldy(tb):
                    for bb, nm in enumerate(('ya', 'yb', 'yc')):
                        P.dma('sp', yT[tb % 2][bb][:], sc[nm].rearrange("(k p) s -> p k s", p=128)[:, :, tb * 512:(tb + 1) * 512], (), [f'yT{tb % 2}_{bb}'])

                def ldg(i):
                    tb_, c_ = divmod(i, 16)
                    P.dma('sp', g3[i % 3][:], gv[:, :, c_, tb_ * 512:(tb_ + 1) * 512], (), [f'g3{i % 3}'])
                ldy(0)
                ldg(0)
                for tb in range(NB):
                    yb_ = tb % 2
                    sl = slice(tb * 512, (tb + 1) * 512)
                    for c in range(16):
                        b = n % 2
                        g3b = n % 3
                        if n + 1 < NB * 16:
                            ldg(n + 1)
                        if c == 2 and tb + 1 < NB:
                            ldy(tb + 1)
                        n += 1
                        for bb in range(3):
                            pb = (b * 3 + bb)
                            for kc in range(8):
                                MM(ps[pb][:], wbr[:, bb, kc, c * 128:(c + 1) * 128], yT[yb_][bb][:, kc, :], kc == 0, kc == 7,
                                   [('wbr', c // 4), f'yT{yb_}_{bb}'], [PK[pb]])
                            TT('dve', mm_[b][bb][:], ps[pb][:], g3[g3b][:, bb, :], ALU.mult, [PK[pb], f'g3{g3b}'], [f'mm{b}_{bb}'])
                        TT('pool', mm_[b][0][:], mm_[b][0][:], mm_[b][1][:], ALU.add, [f'mm{b}_0', f'mm{b}_1'], [f'mm{b}_0'])
                        TT('pool', mo[b][:], mm_[b][0][:], mm_[b][2][:], ALU.add, [f'mm{b}_0', f'mm{b}_2'], [f'mo{b}'])
                        P.dma('act', sc['mT'][c * 128:(c + 1) * 128, sl], mo[b][:], [f'mo{b}'], [('mT', c, tb)])

            P.enabled = True
            sW.__exit__(None, None, None)
            with Scope() as s8:
                P.enabled = (upto >= 8) and (8 not in skip)
                wo = s8.T("wo", [128, 16, D], BF16)
                wov = Wl['wout'].rearrange("p (k n) -> p k n", k=16)
                wst = [s8.T(f"wst{i}", [128, D], F32) for i in range(2)]
                for kc in range(16):
                    P.dma('sp', wst[kc % 2][:], wov[:, kc, :], (), [f'wst{kc % 2}'])
                    CP('act', wo[:, kc, :], wst[kc % 2][:], [f'wst{kc % 2}'], [('wo', kc)])
                mT = [s8.T(f"mTt{i}", [128, 16, 512], BF16) for i in range(2)]
                xr = [s8.T(f"xr{i}", [128, D], F32) for i in range(2)]
                xo = [s8.T(f"xo{i}", [128, D], F32) for i in range(2)]
                mTv = sc['mT'].rearrange("(k p) s -> p k s", p=128)
                n = 0
                for tb in range(NB):
                    mb = tb % 2
                    if tb == 0:
                        P.dma('sp', mT[0][:], mTv[:, :, 0:512], (), ['mTt0'])
                    if tb == 0:
                        P.dma('sp', xr[0][:], x_src[0:128, :], [('xd', 0)], ['xr0'])
                    if tb + 1 < NB:
                        P.dma('sp', mT[(tb + 1) % 2][:], mTv[:, :, (tb + 1) * 512:(tb + 2) * 512], (), [f'mTt{(tb + 1) % 2}'])
                    for j in range(4):
                        tt = tb * 4 + j
                        b = tt % 2
                        if tt + 1 < NT:
                            P.dma('sp', xr[(tt + 1) % 2][:], x_src[(tt + 1) * 128:(tt + 2) * 128, :], [('xd', tt + 1)], [f'xr{(tt + 1) % 2}'])
                        for cbk in range(4):
                            pb = n % 6
                            n += 1
                            for kc in range(16):
                                MM(ps[pb][:], mT[mb][:, kc, j * 128:(j + 1) * 128], wo[:, kc, cbk * 512:(cbk + 1) * 512], kc == 0, kc == 15,
                                   [f'mTt{mb}', ('wo', kc)], [PK[pb]])
                            TT('dve', xo[b][:, cbk * 512:(cbk + 1) * 512], ps[pb][:], xr[b][:, cbk * 512:(cbk + 1) * 512], ALU.add,
                               [PK[pb], f'xr{b}'], [f'xo{b}'])
                        P.dma('act', x_dst[tt * 128:(tt + 1) * 128, :], xo[b][:], [f'xo{b}'], [('xo_d', tt)])
            P.enabled = True
            ls.__exit__(None, None, None)
        gs.__exit__(None, None, None)
        P.barrier()
        P.emit()
    return nc


def _kmajor(w, cw_cols):
    K = w.shape[0]
    sub = w[:, cw_cols]
    return np.ascontiguousarray(sub.reshape(K // 128, 128, sub.shape[1]).transpose(1, 0, 2)).reshape(128, -1)


def prep_layer(inp, l):
    f = np.float32
    w_in = inp['w_in'][l]
    win = np.concatenate([_kmajor(w_in, cols) for (_, _, cols, _, _) in CHUNKS], axis=1)
    wuq = inp['mla_w_uq'][l]
    wukv = inp['mla_w_ukv'][l]
    uq_parts, ukv_parts = [], []
    for h in range(NH):
        b0 = h * 192
        uq_parts += [_kmajor(wuq, np.arange(b0, b0 + 128)), _kmajor(wuq, np.arange(b0 + 128, b0 + 192)),
                     _kmajor(wuq, np.concatenate([np.arange(b0 + 160, b0 + 192), np.arange(b0 + 128, b0 + 160)]))]
        c0 = h * 256
        ukv_parts += [_kmajor(wukv, np.arange(c0, c0 + 128)), _kmajor(wukv, np.arange(c0 + 128, c0 + 256))]
    vecs = np.zeros((128, NVEC), f)
    vecs[:, V_GT:V_GT + 16] = inp['norm_g'][l].reshape(16, 128).T
    vecs[:, V_CQG:V_CQG + 4] = inp['mla_cq_norm'][l].reshape(4, 128).T
    vecs[:, V_CKVG:V_CKVG + 4] = inp['mla_ckv_norm'][l].reshape(4, 128).T
    qg, kg = inp['mla_q_norm'][l], inp['mla_k_norm'][l]
    vecs[:, V_GQN] = qg[0:128]
    vecs[0:64, V_GQA] = qg[128:192]
    vecs[0:64, V_GQB] = np.concatenate([qg[160:192], qg[128:160]])
    vecs[:, V_GKN] = kg[0:128]
    vecs[0:64, V_GKA] = kg[128:192]
    vecs[0:64, V_GKB] = np.concatenate([kg[160:192], kg[128:160]])
    vecs[:, V_OG:V_OG + 2] = inp['gla_o_norm'][l].reshape(2, 128).T
    wbr = inp['w_branch'][l]
    wbr_r = np.ascontiguousarray(wbr.reshape(3, 8, 128, D).transpose(2, 0, 1, 3)).reshape(128, -1)
    wout_r = np.ascontiguousarray(inp['w_out'][l].reshape(16, 128, D).transpose(1, 0, 2)).reshape(128, -1)
    return {
        f"win{l}": np.ascontiguousarray(win, f),
        f"wuq{l}": np.ascontiguousarray(np.concatenate(uq_parts, axis=1), f),
        f"wukv{l}": np.ascontiguousarray(np.concatenate(ukv_parts, axis=1), f),
        f"vecs{l}": vecs,
        f"vgb{l}": np.ascontiguousarray(np.broadcast_to(inp['sgu_v_norm'][l][None, :], (128, 1024)), f),
        f"wsT{l}": np.ascontiguousarray(inp['sgu_w_s'][l].transpose(2, 0, 1).reshape(128, 1024), f),
        f"bs{l}": np.ascontiguousarray(inp['sgu_b_s'][l].reshape(1, 1024), f),
        f"wa2{l}": np.ascontiguousarray(inp['gla_w_a2'][l], f),
        f"ba{l}": np.ascontiguousarray(inp['gla_b_a'][l].reshape(1, 512), f),
        f"wbr{l}": np.ascontiguousarray(wbr_r, f),
        f"wout{l}": np.ascontiguousarray(wout_r, f),
    }


def consts():
    f = np.float32
    s = np.arange(128)
    U = (s[:, None] <= s[None, :]).astype(f)
    Urev = (s[:, None] > s[None, :]).astype(f)
    ones = np.ones((128, 128), f)
    misc = np.zeros((128, 4), f)
    inv_freq = (1.0 / (10000.0 ** (np.arange(0, 64, 2, dtype=np.float32) / np.float32(64)))).astype(f)
    misc[0:64, 0] = np.concatenate([inv_freq, inv_freq])
    misc[0:64, 1] = np.concatenate([-np.ones(32, f), np.ones(32, f)])
    cf = np.concatenate([U, Urev, ones, np.tile(U, (1, 4)), misc], axis=1)
    cb = np.concatenate([np.eye(128, dtype=f), U, ones], axis=1).astype(ml_dtypes.bfloat16)
    return np.ascontiguousarray(cf, f), np.ascontiguousarray(cb)


_NC_CACHE = {}


def run(inp, S, depth, n_cores):
    key = (S, depth)
    if key not in _NC_CACHE:
        _NC_CACHE[key] = build(S, depth)
    nc = _NC_CACHE[key]
    cf, cb = consts()
    shared = {"cf": cf, "cb": cb}
    for l in range(depth):
        shared.update(prep_layer(inp, l))
    in_maps = []
    for b in range(n_cores):
        m = dict(shared)
        m["x"] = np.ascontiguousarray(inp['x'][b], np.float32)
        m["posr"] = np.ascontiguousarray(np.broadcast_to(inp['positions'][b][None, :], (64, S)), np.int32)
        in_maps.append(m)
    res = run_bass_kernel_spmd(nc, in_maps, core_ids=list(range(n_cores)))
    return np.stack([res.results[b]["out"] for b in range(n_cores)], axis=0)


def kernel(**inputs):
    inp = {k: np.asarray(v) for k, v in inputs.items()}
    return run(inp, 4096, DEPTH, 8).astype(np.float32)
```

```python
import os
import numpy as np
import ml_dtypes
from contextlib import ExitStack
import concourse.bass as bass
import concourse.mybir as mybir
from concourse.bass_utils import run_bass_kernel_spmd

F32 = mybir.dt.float32
BF16 = mybir.dt.bfloat16
I32 = mybir.dt.int32
AF = mybir.ActivationFunctionType
ALU = mybir.AluOpType

D = 2048
DEPTH = 2
NH = 8
EPS = 1e-6
IN_COLS = 14416
TWO_PI = 6.283185
GCUT = int(os.environ.get('GCUT', '99'))
HS_ENG = os.environ.get('HS_ENG', 'dve')

ENGS = ['pe', 'act', 'dve', 'pool', 'sp']
EPOCH = 24000
NDMA = 12


class Prog:
    def __init__(self, nc, stack):
        self.nc = nc
        self.stack = stack
        self.lists = {e: [] for e in ENGS}
        self.cnt = {e: 0 for e in ENGS}
        self.cursem = {}
        self.allsems = []
        for e in ENGS:
            self.cursem[e] = self._newsem(f"c_{e}")
        self.known = {e: {} for e in ENGS}
        self.lastw = {}
        self.readers = {}
        self.dmasems = {}
        self.dmacnt = {}
        self.dmaidx = {}
        for q in ('sp', 'act', 'pool'):
            self.dmasems[q] = [self._newsem(f"d_{q}{i}") for i in range(NDMA)]
            self.dmacnt[q] = [0] * NDMA
            self.dmaidx[q] = 0
        self.outstanding = {}
        self.enabled = True

    def _newsem(self, name):
        s = self.stack.enter_context(self.nc.semaphore(name + f"_{len(self.allsems)}"))
        self.allsems.append(s)
        return s

    def _need(self, eng, reads, writes, extra=()):
        need = {}

        def add(tok):
            if tok is None:
                return
            s, v = tok
            if need.get(s, 0) < v:
                need[s] = v
        for r in reads:
            add(self.lastw.get(r))
        for w in writes:
            add(self.lastw.get(w))
            for s, v in self.readers.get(w, {}).items():
                add((s, v))
        for t in extra:
            add(t)
        waits = []
        kn = self.known[eng]
        for s, v in need.items():
            if eng == 'pe' and s is self.cursem['pe']:
                continue
            if kn.get(s, 0) >= v:
                continue
            kn[s] = v
            waits.append((s, v))
        return waits

    def _commit(self, tok, reads, writes):
        s, v = tok
        for r in reads:
            d = self.readers.setdefault(r, {})
            if d.get(s, 0) < v:
                d[s] = v
        for w in writes:
            self.lastw[w] = tok
            self.readers[w] = {}
        self.outstanding[s] = v

    def op(self, eng, fn, reads=(), writes=()):
        if not self.enabled:
            return None
        waits = self._need(eng, reads, writes)
        if self.cnt[eng] >= EPOCH:
            self.cursem[eng] = self._newsem(f"c_{eng}")
            self.cnt[eng] = 0
        self.cnt[eng] += 1
        tok = (self.cursem[eng], self.cnt[eng])
        self.lists[eng].append((waits, fn, (tok[0], 1)))
        self._commit(tok, reads, writes)
        return tok

    def dma(self, q, out, in_, reads=(), writes=()):
        if not self.enabled:
            return None
        i = self.dmaidx[q]
        self.dmaidx[q] = (i + 1) % NDMA
        sem = self.dmasems[q][i]
        prev = self.dmacnt[q][i]
        extra = [(sem, prev)] if prev > 0 else []
        waits = self._need(q, reads, writes, extra)
        if prev + 16 > EPOCH:
            sem = self._newsem(f"d_{q}{i}")
            self.dmasems[q][i] = sem
            prev = 0
        self.dmacnt[q][i] = prev + 16
        tok = (sem, prev + 16)

        def fn(e, out=out, in_=in_):
            return e.dma_start(out=out, in_=in_)
        self.lists[q].append((waits, fn, (sem, 16)))
        self._commit(tok, reads, writes)
        return tok

    def barrier(self):
        for e in ENGS:
            kn = self.known[e]
            waits = []
            for s, v in self.outstanding.items():
                if kn.get(s, 0) >= v:
                    continue
                kn[s] = v
                waits.append((s, v))
            if waits:
                self.lists[e].append((waits, None, None))
        self.lastw = {}
        self.readers = {}

    def emit(self):
        nc = self.nc
        engmap = {'pe': 'tensor', 'act': 'scalar', 'dve': 'vector', 'pool': 'gpsimd', 'sp': 'sync'}
        with nc.Block() as block:
            for e in ENGS:
                lst = self.lists[e]

                def body(eng, lst=lst):
                    for waits, fn, inc in lst:
                        for s, v in waits:
                            eng.wait_ge(s, v)
                        if fn is not None:
                            ins = fn(eng)
                            ins.then_inc(inc[0], inc[1])
                getattr(block, engmap[e])(body)


def win_chunks():
    ch = []
    def fm(name, c0, n, cw=128):
        for i in range(n // cw):
            ch.append((name, i, np.arange(c0 + i * cw, c0 + (i + 1) * cw), cw, 'fm'))
    def tm(name, c0, n):
        for i in range(n // 512):
            ch.append((name, i, np.arange(c0 + i * 512, c0 + (i + 1) * 512), 512, 'tm'))
    fm('cq', 0, 512)
    fm('ckv', 512, 512)
    ch.append(('krA', 0, np.arange(1024, 1088), 64, 'fm'))
    ch.append(('krB', 0, np.concatenate([np.arange(1056, 1088), np.arange(1024, 1056)]), 64, 'fm'))
    ch.append(('ar', 0, np.arange(7232, 7248), 16, 'fm'))
    tm('kctok', 5696, 512)
    fm('qc', 5184, 512)
    fm('kc', 5696, 512)
    ch.append(('vc', 0, np.arange(6208, 6720), 512, 'tm'))
    fm('za', 1088, 1024)
    ch.append(('vc', 1, np.arange(6720, 7232), 512, 'tm'))
    fm('zb', 4160, 1024)
    ch.append(('vb', 0, np.arange(3136, 3648), 512, 'tm'))
    fm('zc', 7248, 1024)
    ch.append(('vb', 1, np.arange(3648, 4160), 512, 'tm'))
    fm('ub', 2112, 1024)
    fm('g', 8272, 6144)
    return ch


CHUNKS = win_chunks()
WIN_R_COLS = 16 * sum(c[3] for c in CHUNKS)

V_GT, V_CQG, V_CKVG, V_GQN, V_GQA, V_GQB, V_GKN, V_GKA, V_GKB, V_OG, NVEC = 0, 16, 20, 24, 25, 26, 27, 28, 29, 30, 32


def build(S, depth, upto=99, skip=()):
    NT = S // 128
    NB = S // 512
    nc = bass.Bass("TRN2", target_bir_lowering=False)
    dram_in = lambda name, shape, dt: nc.dram_tensor(name, shape, dt, kind="ExternalInput").ap()
    dram_sc = lambda name, shape, dt: nc.dram_tensor(name, shape, dt).ap()
    x_in = dram_in("x", [S, D], F32)
    posr = dram_in("posr", [64, S], I32)
    cf_d = dram_in("cf", [128, 128 * 3 + 512 + 4], F32)
    cb_d = dram_in("cb", [128, 384], BF16)
    W = []
    for l in range(depth):
        W.append(dict(
            win=dram_in(f"win{l}", [128, WIN_R_COLS], F32),
            wuq=dram_in(f"wuq{l}", [128, NH * 1024], F32),
            wukv=dram_in(f"wukv{l}", [128, NH * 1024], F32),
            vecs=dram_in(f"vecs{l}", [128, NVEC], F32),
            vgb=dram_in(f"vgb{l}", [128, 1024], F32),
            wsT=dram_in(f"wsT{l}", [128, 1024], F32),
            bs=dram_in(f"bs{l}", [1, 1024], F32),
            wa2=dram_in(f"wa2{l}", [16, 512], F32),
            ba=dram_in(f"ba{l}", [1, 512], F32),
            wbr=dram_in(f"wbr{l}", [128, 3 * 8 * D], F32),
            wout=dram_in(f"wout{l}", [128, 16 * D], F32),
        ))
    out_d = nc.dram_tensor("out", [S, D], F32, kind="ExternalOutput").ap()
    xmid = dram_sc("xmid", [S, D], F32)
    sc = dict(
        cq=dram_sc("s_cq", [512, S], BF16), ckv=dram_sc("s_ckv", [512, S], BF16),
        krA=dram_sc("s_krA", [64, S], BF16), krB=dram_sc("s_krB", [64, S], BF16),
        ar=dram_sc("s_ar", [16, S], BF16),
        qc=dram_sc("s_qc", [512, S], BF16), kc=dram_sc("s_kc", [512, S], BF16),
        kctok=dram_sc("s_kctok", [S, 512], BF16), vc=dram_sc("s_vc", [S, 1024], BF16),
        za=dram_sc("s_za", [1024, S], BF16), zb=dram_sc("s_zb", [1024, S], BF16), zc=dram_sc("s_zc", [1024, S], BF16),
        ub=dram_sc("s_ub", [1024, S], BF16), vb=dram_sc("s_vb", [S, 1024], BF16),
        g=dram_sc("s_g", [6144, S], BF16),
        ya=dram_sc("s_ya", [1024, S], BF16), yb=dram_sc("s_yb", [1024, S], BF16), yc=dram_sc("s_yc", [1024, S], BF16),
        mT=dram_sc("s_mT", [D, S], BF16),
    )

    with ExitStack() as st:
        P = Prog(nc, st)

        def MM(out, lhsT, rhs, s, e, r, w):
            P.op('pe', lambda en: en.matmul(out, lhsT, rhs, start=s, stop=e), r, w)

        def ACT(out, in_, func, r, w, **kw):
            P.op('act', lambda en: en.activation(out, in_, func, **kw), r, w)

        def TT(eng, out, a, b, op, r, w):
            P.op(eng, lambda en: en.tensor_tensor(out, a, b, op), r, w)

        def TS(eng, out, a, s1, s2, op0, op1, r, w):
            if s2 is None:
                P.op(eng, lambda en: en.tensor_scalar(out, a, s1, None, op0), r, w)
            else:
                P.op(eng, lambda en: en.tensor_scalar(out, a, s1, s2, op0, op1), r, w)

        def STT(eng, out, a, s, b, op0, op1, r, w):
            P.op(eng, lambda en: en.scalar_tensor_tensor(out, a, s, b, op0, op1), r, w)

        def CP(eng, out, a, r, w):
            if eng == 'act':
                P.op(eng, lambda en: en.activation(out, a, AF.Copy), r, w)
            else:
                P.op(eng, lambda en: en.tensor_copy(out, a), r, w)

        def MEMSET(eng, out, val, w):
            P.op(eng, lambda en: en.memset(out, val), (), w)

        def RECIP(out, a, r, w):
            P.op('dve', lambda en: en.reciprocal(out, a), r, w)

        def rstd_from(out, ssum, n, r, w, tmp, tmpk):
            ACT(tmp, ssum, AF.Ln, list(r), [tmpk], scale=1.0 / n, bias=EPS)
            ACT(out, tmp, AF.Exp, [tmpk], list(w), scale=-0.5)

        uid = [0]

        class Scope:
            def __init__(self):
                self.es = ExitStack()
            def __enter__(self):
                self.es.__enter__()
                return self
            def __exit__(self, *a):
                P.barrier()
                return self.es.__exit__(*a)
            def T(self, name, shape, dt):
                uid[0] += 1
                return self.es.enter_context(nc.sbuf_tensor(f"t{uid[0]}_{name}", shape, dt))

        gs = Scope()
        gs.__enter__()
        cf = gs.T("cf", [128, 128 * 3 + 512 + 4], F32)
        cb = gs.T("cbt", [128, 384], BF16)
        P.dma('sp', cf[:], cf_d, (), ['cf'])
        P.dma('sp', cb[:], cb_d, (), ['cb'])
        U_f = cf[:, 0:128]
        Urev_f = cf[:, 128:256]
        ones_f = cf[:, 256:384]
        U4_f = cf[:, 384:896]
        invf2 = cf[0:64, 896:897]
        sgn = cf[0:64, 897:898]
        ident_b = cb[:, 0:128]
        U_b = cb[:, 128:256]
        ones_b = cb[:, 256:384]
        ps = [st.enter_context(nc.psum_tensor(f"ps{i}", [128, 512], F32)) for i in range(8)]
        ptv = [ps[6][:].bitcast(BF16), ps[7][:].bitcast(BF16)]
        PK = [f"ps{i}" for i in range(8)]

        def rope_tables(cos2, sin2):
            s0 = Scope()
            s0.__enter__()
            pi_t = s0.T("pos_i", [64, S], I32)
            u_t = s0.T("rp_u", [64, S], F32)
            r_i = s0.T("rp_ri", [64, S], I32)
            r_f = s0.T("rp_rf", [64, S], F32)
            m_t = s0.T("rp_m", [64, S], F32)
            P.dma('sp', pi_t[:], posr, (), ['pos_i'])
            CP('dve', u_t[:], pi_t[:], ['pos_i'], ['u'])
            TS('dve', u_t[:], u_t[:], invf2, 1.0 / (2 * np.pi), ALU.mult, ALU.mult, ['u', 'cf'], ['u'])
            for which, dst in (('sin', sin2), ('cos', cos2)):
                if which == 'cos':
                    TS('dve', u_t[:], u_t[:], 0.25, None, ALU.add, None, ['u'], ['u'])
                CP('dve', r_i[:], u_t[:], ['u'], ['ri'])
                CP('dve', r_f[:], r_i[:], ['ri'], ['rf'])
                TT('dve', r_f[:], u_t[:], r_f[:], ALU.subtract, ['u', 'rf'], ['rf'])
                TS('dve', m_t[:], r_f[:], 0.5, None, ALU.is_gt, None, ['rf'], ['m'])
                TT('dve', r_f[:], r_f[:], m_t[:], ALU.subtract, ['rf', 'm'], ['rf'])
                TS('dve', m_t[:], r_f[:], -0.5, None, ALU.is_lt, None, ['rf'], ['m'])
                TT('dve', r_f[:], r_f[:], m_t[:], ALU.add, ['rf', 'm'], ['rf'])
                ACT(dst[:], r_f[:], AF.Sin, ['rf'], [which], scale=TWO_PI)
            TS('dve', sin2[:], sin2[:], sgn, None, ALU.mult, None, ['sin', 'cf'], ['sin'])
            en_ = P.enabled
            P.enabled = True
            s0.__exit__(None, None, None)
            P.enabled = en_

        for l in range(depth):
            Wl = W[l]
            x_src = x_in if l == 0 else xmid
            x_dst = out_d if l == depth - 1 else xmid
            ls = Scope()
            ls.__enter__()
            vecs = ls.T("vecs", [128, NVEC], F32)
            P.dma('sp', vecs[:], Wl['vecs'], (), ['vecs'])

            with Scope() as s12:
                hT = s12.T("hT", [128, 16, S], BF16)
                with Scope() as s1:
                    P.enabled = (upto >= 1) and (1 not in skip)
                    xt = [s1.T(f"xt{i}", [128, D], F32) for i in range(2)]
                    hs = [s1.T(f"hs{i}", [128, D], BF16) for i in range(2)]
                    junk = s1.T("junk", [128, D], BF16)
                    ss = [s1.T(f"ss{i}", [128, 1], F32) for i in range(2)]
                    sq = [s1.T(f"sq{i}", [128, 1], F32) for i in range(2)]
                    rs = [s1.T(f"rs{i}", [128, 1], F32) for i in range(2)]
                    G = s1.T("G", [128, 16, 128], F32)
                    for kc in range(16):
                        TS('dve', G[:, kc, :], ones_f, vecs[:, V_GT + kc:V_GT + kc + 1], None, ALU.mult, None, ['cf', 'vecs'], ['G'])
                    P.dma('sp', xt[0][:], x_src[0:128, :], [('xd', 0)], ['xt0'])
                    for tt in range(NT):
                        b = tt % 2
                        if tt + 1 < NT:
                            P.dma('sp', xt[(tt + 1) % 2][:], x_src[(tt + 1) * 128:(tt + 2) * 128, :], [('xd', tt + 1)], [f'xt{(tt + 1) % 2}'])
                        MEMSET('pool', ss[b][:], 0.0, [f'ss{b}'])
                        ACT(junk[:], xt[b][:], AF.Square, [f'xt{b}', f'ss{b}'], ['junk', f'ss{b}'], accum_out=ss[b][:])
                        rstd_from(rs[b][:], ss[b][:], D, [f'ss{b}'], [f'rs{b}'], sq[b][:], f'sq{b}')
                        TS('dve', hs[b][:, 0:1024], xt[b][:, 0:1024], rs[b][:, 0:1], None, ALU.mult, None, [f'xt{b}', f'rs{b}'], [f'hs{b}a'])
                        TS(HS_ENG, hs[b][:, 1024:2048], xt[b][:, 1024:2048], rs[b][:, 0:1], None, ALU.mult, None, [f'xt{b}', f'rs{b}'], [f'hs{b}b'])
                        for kq in range(4):
                            half = (tt * 4 + kq) % 2
                            for j in range(4):
                                kc = kq * 4 + j
                                P.op('pe', (lambda o, i: (lambda en: en.transpose(o, i, ident_b)))(
                                    ptv[half][:, j * 128:(j + 1) * 128], hs[b][:, kc * 128:(kc + 1) * 128]),
                                    [f'hs{b}a' if kq < 2 else f'hs{b}b', 'cb'], [PK[6 + half]])
                            TT('dve', hT[:, kq * 4:(kq + 1) * 4, tt * 128:(tt + 1) * 128], ptv[half][:, 0:512].rearrange("p (j t) -> p j t", t=128),
                               G[:, kq * 4:(kq + 1) * 4, :], ALU.mult, [PK[6 + half], 'G'], [('hT', tt)])
                with Scope() as s2:
                    P.enabled = (upto >= 2) and (2 not in skip)
                    wf = [s2.T(f"wf{i}", [128, 16, 128], BF16) for i in range(3)]
                    wt = [s2.T(f"wt{i}", [128, 16, 512], BF16) for i in range(1)]
                    ob = [s2.T(f"ob{i}", [128, 512], BF16) for i in range(4)]
                    nfm = 0
                    ntm = 0
                    nps = 0
                    nob = 0
                    off = 0
                    for (name, idx, cols, cw, mode) in CHUNKS:
                        src = Wl['win'][:, 16 * off:16 * (off + cw)].rearrange("p (k c) -> p k c", c=cw)
                        off += cw
                        if mode == 'fm':
                            wb = nfm % 3
                            nfm += 1
                            P.dma('pool', wf[wb][:, :, 0:cw], src, (), [f'wf{wb}'])
                            func = {'za': AF.Silu, 'zb': AF.Silu, 'zc': AF.Silu, 'ub': AF.Gelu_apprx_tanh, 'g': AF.Sigmoid}.get(name)
                            for tb in range(NB):
                                pb = nps % 6
                                nps += 1
                                for kc in range(16):
                                    MM(ps[pb][0:cw, :], wf[wb][:, kc, 0:cw], hT[:, kc, tb * 512:(tb + 1) * 512], kc == 0, kc == 15,
                                       [f'wf{wb}'] + [('hT', tb * 4 + i) for i in range(4)], [PK[pb]])
                                o = nob % 4
                                nob += 1
                                if func is not None:
                                    ACT(ob[o][0:cw, :], ps[pb][0:cw, :], func, [PK[pb]], [f'ob{o}'])
                                else:
                                    CP('dve', ob[o][0:cw, :], ps[pb][0:cw, :], [PK[pb]], [f'ob{o}'])
                                P.dma('sp', sc[name][idx * cw:(idx + 1) * cw, tb * 512:(tb + 1) * 512], ob[o][0:cw, :], [f'ob{o}'], [(name, idx, tb)])
                        else:
                            wb = 0
                            ntm += 1
                            P.dma('pool', wt[wb][:], src, (), [f'wt{wb}'])
                            func = AF.Gelu_apprx_tanh if name == 'vb' else None
                            for tt in range(NT):
                                pb = nps % 6
                                nps += 1
                                for kc in range(16):
                                    MM(ps[pb][:], hT[:, kc, tt * 128:(tt + 1) * 128], wt[wb][:, kc, :], kc == 0, kc == 15,
                                       [f'wt{wb}', ('hT', tt)], [PK[pb]])
                                o = nob % 4
                                nob += 1
                                if func is not None:
                                    ACT(ob[o][:], ps[pb][:], func, [PK[pb]], [f'ob{o}'])
                                else:
                                    CP('dve', ob[o][:], ps[pb][:], [PK[pb]], [f'ob{o}'])
                                P.dma('sp', sc[name][tt * 128:(tt + 1) * 128, idx * 512:(idx + 1) * 512], ob[o][:], [f'ob{o}'], [(name, idx, tt)])

            with Scope() as s3:
                P.enabled = (upto >= 3) and (3 not in skip)
                cos2 = s3.T("cos2", [64, S], F32)
                sin2 = s3.T("sin2", [64, S], F32)
                rope_tables(cos2, sin2)
                cqn = s3.T("cqn", [128, 4, S], BF16)
                ckvn = s3.T("ckvn", [128, 4, S], BF16)
                krA = s3.T("krA", [64, S], BF16)
                krB = s3.T("krB", [64, S], BF16)
                P.dma('sp', krA[:], sc['krA'], (), ['krA'])
                P.dma('sp', krB[:], sc['krB'], (), ['krB'])
                tmpa = [s3.T(f"tmpa{i}", [128, 512], F32) for i in range(2)]
                rst = [s3.T(f"rst{i}", [128, 512], F32) for i in range(2)]
                s3p = Scope()
                s3p.__enter__()
                sqt = [s3p.T(f"sqt{i}", [128, 4, 512], BF16) for i in range(2)]
                cin = [s3p.T(f"cin{i}", [128, 4, 512], BF16) for i in range(2)]
                n = 0
                for (nm, dst, gcol) in (('cq', cqn, V_CQG), ('ckv', ckvn, V_CKVG)):
                    srcv = sc[nm].rearrange("(k p) s -> p k s", p=128)
                    for tb in range(NB):
                        b = n % 2
                        n += 1
                        P.dma('sp', cin[b][:], srcv[:, :, tb * 512:(tb + 1) * 512], (), [f'cin{b}'])
                        ACT(sqt[b][:], cin[b][:], AF.Square, [f'cin{b}'], [f'sqt{b}'])
                        for kc in range(4):
                            MM(ps[b][:], ones_b, sqt[b][:, kc, :], kc == 0, kc == 3, ['cb', f'sqt{b}'], [PK[b]])
                        rstd_from(rst[b][:], ps[b][:], 512, [PK[b]], [f'rst{b}'], tmpa[b][:], f'tmpa{b}')
                        for kc in range(4):
                            STT('dve', dst[:, kc, tb * 512:(tb + 1) * 512], cin[b][:, kc, :], vecs[:, gcol + kc:gcol + kc + 1], rst[b][:],
                                ALU.mult, ALU.mult, [f'cin{b}', 'vecs', f'rst{b}'], [(nm + 'n', tb)])
                en_ = P.enabled
                P.enabled = True
                s3p.__exit__(None, None, None)
                P.enabled = en_
                Qn = s3.T("Qn", [128, S], BF16)
                Qr = s3.T("Qr", [64, S], BF16)
                Kn = s3.T("Kn", [128, S], BF16)
                Kr = s3.T("Kr", [64, S], BF16)
                Vh = s3.T("Vh", [128, NT, 128], BF16)
                wq = s3.T("wq", [128, 1024], BF16)
                wkv = s3.T("wkv", [128, 1024], BF16)
                sqn = [s3.T(f"sqn{i}", [128, 512], BF16) for i in range(4)]
                sqa = [s3.T(f"sqa{i}", [64, 512], BF16) for i in range(4)]
                Ar = [s3.T(f"Ar{i}", [64, 512], F32) for i in range(2)]
                Br = [s3.T(f"Br{i}", [64, 512], F32) for i in range(2)]
                An = [s3.T(f"An{i}", [64, 512], F32) for i in range(2)]
                Bn = [s3.T(f"Bn{i}", [64, 512], F32) for i in range(2)]
                PT = [s3.T(f"PT{i}", [128, 512], BF16) for i in range(4)]
                zat = [s3.T(f"zat{i}", [128, 512], BF16) for i in range(2)]
                rL = [s3.T("rL0", [128, 512], F32)] * 2
                yf = [s3.T(f"yf{i}", [128, 512], F32) for i in range(2)]
                yo = [s3.T(f"yo{i}", [128, 512], BF16) for i in range(2)]
                SCALE = 192 ** -0.5
                it = 0
                for h in range(NH):
                    P.dma('pool', wq[:], Wl['wuq'][:, h * 1024:(h + 1) * 1024], (), ['wq'])
                    P.dma('pool', wkv[:], Wl['wukv'][:, h * 1024:(h + 1) * 1024], (), ['wkv'])
                    wq_n = lambda kc: wq[:, kc * 128:(kc + 1) * 128]
                    wq_a = lambda kc: wq[:, 512 + kc * 64:512 + (kc + 1) * 64]
                    wq_b = lambda kc: wq[:, 768 + kc * 64:768 + (kc + 1) * 64]
                    wk_n = lambda kc: wkv[:, kc * 128:(kc + 1) * 128]
                    wv = lambda kc: wkv[:, 512 + kc * 128:512 + (kc + 1) * 128]
                    def Pst(tb):
                        sl = slice(tb * 512, (tb + 1) * 512)
                        par = tb % 2
                        pqn, kqn = ps[0 + par], PK[0 + par]
                        pkn, kkn = ps[2 + par], PK[2 + par]
                        for kc in range(4):
                            MM(pqn[:], wq_n(kc), cqn[:, kc, sl], kc == 0, kc == 3, ['wq', ('cqn', tb)], [kqn])
                        for kc in range(4):
                            MM(ps[4][0:64, :], wq_a(kc), cqn[:, kc, sl], kc == 0, kc == 3, ['wq', ('cqn', tb)], [PK[4]])
                        for kc in range(4):
                            MM(ps[5][0:64, :], wq_b(kc), cqn[:, kc, sl], kc == 0, kc == 3, ['wq', ('cqn', tb)], [PK[5]])
                        for kc in range(4):
                            MM(pkn[:], wk_n(kc), ckvn[:, kc, sl], kc == 0, kc == 3, ['wkv', ('ckvn', tb)], [kkn])
                        for j in range(4):
                            tt = tb * 4 + j
                            for kc in range(4):
                                MM(ps[6][:, j * 128:(j + 1) * 128], ckvn[:, kc, tt * 128:(tt + 1) * 128], wv(kc), kc == 0, kc == 3,
                                   ['wkv', ('ckvn', tb)], [PK[6]])
                        bq_, bk_ = par * 2, par * 2 + 1
                        ACT(sqn[bq_][:], pqn[:], AF.Square, [kqn], [f'sqn{bq_}'])
                        ACT(sqa[bq_][:], ps[4][0:64, :], AF.Square, [PK[4]], [f'sqa{bq_}'])
                        ACT(Ar[par][:], ps[4][0:64, :], AF.Copy, [PK[4]], [f'Ar{par}'])
                        ACT(Br[par][:], ps[5][0:64, :], AF.Copy, [PK[5]], [f'Br{par}'])
                        ACT(sqn[bk_][:], pkn[:], AF.Square, [kkn], [f'sqn{bk_}'])
                        ACT(sqa[bk_][:], krA[:, sl], AF.Square, ['krA'], [f'sqa{bk_}'])
                        CP('act', Vh[:, tb * 4:(tb + 1) * 4, :], ps[6][:].rearrange("p (j d) -> p j d", d=128), [PK[6]], [('Vh', tb)])

                    def Sst(tb):
                        sl = slice(tb * 512, (tb + 1) * 512)
                        par = tb % 2
                        pqn, kqn = ps[0 + par], PK[0 + par]
                        pkn, kkn = ps[2 + par], PK[2 + par]
                        bq_, bk_ = par * 2, par * 2 + 1
                        b = 0
                        MM(ps[7][:], ones_b, sqn[bq_][:], True, False, ['cb', f'sqn{bq_}'], [PK[7]])
                        MM(ps[7][:], cb[0:64, 256:384], sqa[bq_][:], False, True, ['cb', f'sqa{bq_}'], [PK[7]])
                        rstd_from(rst[b][:], ps[7][:], 192, [PK[7]], [f'rst{b}'], tmpa[b][:], f'tmpa{b}')
                        STT('dve', Qn[:, sl], pqn[:], vecs[:, V_GQN:V_GQN + 1], rst[b][:], ALU.mult, ALU.mult, [kqn, 'vecs', f'rst{b}'], [('Qn', tb)])
                        STT('dve', An[b][:], Ar[par][:], vecs[0:64, V_GQA:V_GQA + 1], rst[b][0:64, :], ALU.mult, ALU.mult, [f'Ar{par}', 'vecs', f'rst{b}'], [f'An{b}'])
                        STT('dve', Bn[b][:], Br[par][:], vecs[0:64, V_GQB:V_GQB + 1], rst[b][0:64, :], ALU.mult, ALU.mult, [f'Br{par}', 'vecs', f'rst{b}'], [f'Bn{b}'])
                        TT('pool', An[b][:], An[b][:], cos2[:, sl], ALU.mult, [f'An{b}', 'cos'], [f'An{b}'])
                        TT('pool', Bn[b][:], Bn[b][:], sin2[:, sl], ALU.mult, [f'Bn{b}', 'sin'], [f'Bn{b}'])
                        TT('pool', Qr[:, sl], An[b][:], Bn[b][:], ALU.add, [f'An{b}', f'Bn{b}'], [('Qr', tb)])
                        b = 1
                        MM(ps[7][:], ones_b, sqn[bk_][:], True, False, ['cb', f'sqn{bk_}'], [PK[7]])
                        MM(ps[7][:], cb[0:64, 256:384], sqa[bk_][:], False, True, ['cb', f'sqa{bk_}'], [PK[7]])
                        rstd_from(rst[b][:], ps[7][:], 192, [PK[7]], [f'rst{b}'], tmpa[b][:], f'tmpa{b}')
                        STT('dve', Kn[:, sl], pkn[:], vecs[:, V_GKN:V_GKN + 1], rst[b][:], ALU.mult, ALU.mult, [kkn, 'vecs', f'rst{b}'], [('Kn', tb)])
                        STT('dve', An[b][:], krA[:, sl], vecs[0:64, V_GKA:V_GKA + 1], rst[b][0:64, :], ALU.mult, ALU.mult, ['krA', 'vecs', f'rst{b}'], [f'An{b}'])
                        STT('dve', Bn[b][:], krB[:, sl], vecs[0:64, V_GKB:V_GKB + 1], rst[b][0:64, :], ALU.mult, ALU.mult, ['krB', 'vecs', f'rst{b}'], [f'Bn{b}'])
                        TT('pool', An[b][:], An[b][:], cos2[:, sl], ALU.mult, [f'An{b}', 'cos'], [f'An{b}'])
                        TT('pool', Bn[b][:], Bn[b][:], sin2[:, sl], ALU.mult, [f'Bn{b}', 'sin'], [f'Bn{b}'])
                        TT('pool', Kr[:, sl], An[b][:], Bn[b][:], ALU.add, [f'An{b}', f'Bn{b}'], [('Kr', tb)])
                    Pst(0)
                    for tb in range(NB):
                        if tb + 1 < NB:
                            Pst(tb + 1)
                        Sst(tb)
                    for qb in range(NB):
                        ob_ = qb % 2
                        pO, pL = ps[3 + ob_ * 2], ps[4 + ob_ * 2]
                        kO, kL = PK[3 + ob_ * 2], PK[4 + ob_ * 2]
                        P.dma('sp', zat[ob_][:], sc['za'][h * 128:(h + 1) * 128, qb * 512:(qb + 1) * 512], (), [f'zat{ob_}'])
                        nk = 4 * qb + 4
                        SB = [0, 1, 2, 7]

                        def s_part(kt):
                            j = max(0, kt - 4 * qb)
                            c0 = j * 128
                            sb_ = kt % 4
                            pS, kS = ps[SB[sb_]], PK[SB[sb_]]
                            qs = slice(qb * 512 + c0, (qb + 1) * 512)
                            ks = slice(kt * 128, (kt + 1) * 128)
                            MM(pS[:, c0:512], Kn[:, ks], Qn[:, qs], True, False, [('Kn', kt // 4), ('Qn', qb)], [kS])
                            MM(pS[:, c0:512], Kr[:, ks], Qr[:, qs], False, True, [('Kr', kt // 4), ('Qr', qb)], [kS])
                            ACT(PT[sb_][:, c0:512], pS[:, c0:512], AF.Exp, [kS], [f'PT{sb_}'], scale=SCALE)
                            if kt >= 4 * qb:
                                TT('pool', PT[sb_][:, c0:c0 + 128], PT[sb_][:, c0:c0 + 128], U_b, ALU.mult, [f'PT{sb_}', 'cb'], [f'PT{sb_}'])

                        def pv_part(kt):
                            j = max(0, kt - 4 * qb)
                            c0 = j * 128
                            sb_ = kt % 4
                            MM(pO[:, c0:512], Vh[:, kt, :], PT[sb_][:, c0:512], kt == 0, kt == nk - 1, [('Vh', kt // 4), f'PT{sb_}'], [kO])
                            MM(pL[:, c0:512], ones_b, PT[sb_][:, c0:512], kt == 0, kt == nk - 1, ['cb', f'PT{sb_}'], [kL])
                        LOOK = 2
                        for kt in range(nk + LOOK):
                            if kt < nk:
                                s_part(kt)
                            if kt >= LOOK:
                                pv_part(kt - LOOK)
                        RECIP(rL[0][:], pL[:], [kL], ['rL0'])
                        TT('dve', yf[ob_][:], pO[:], rL[0][:], ALU.mult, [kO, 'rL0'], [f'yf{ob_}'])
                        TT('pool', yo[ob_][:], yf[ob_][:], zat[ob_][:], ALU.mult, [f'yf{ob_}', f'zat{ob_}'], [f'yo{ob_}'])
                        P.dma('sp', sc['ya'][h * 128:(h + 1) * 128, qb * 512:(qb + 1) * 512], yo[ob_][:], [f'yo{ob_}'], [('ya', h, qb)])

            sW = Scope()
            sW.__enter__()
            wbr = sW.T("wbr", [128, 3, 8, D], BF16)
            wbv = Wl['wbr'].rearrange("p (b k n) -> p b k n", b=3, k=8)
            wbr_jobs = [(cg, bb, kc) for cg in range(4) for bb in range(3) for kc in range(8)]

            def wbr_prefetch(n_):
                en_ = P.enabled
                P.enabled = (upto >= 7) and (7 not in skip)
                for _ in range(n_):
                    if wbr_jobs:
                        cg, bb, kc = wbr_jobs.pop(0)
                        P.dma('pool', wbr[:, bb, kc, cg * 512:(cg + 1) * 512], wbv[:, bb, kc, cg * 512:(cg + 1) * 512], (), [('wbr', cg)])
                P.enabled = en_
            with Scope() as s5:
                P.enabled = (upto >= 5) and (5 not in skip)
                vn = s5.T("vn", [128, NT, 1024], BF16)
                vgb = s5.T("vgb", [128, 1024], F32)
                wsT = s5.T("wsT", [128, 8, 128], F32)
                wsTb = s5.T("wsTb", [128, 8, 128], BF16)
                bsf = s5.T("bsf", [1, 1024], F32)
                P.dma('sp', vgb[:], Wl['vgb'], (), ['vgb'])
                P.dma('sp', wsT[:], Wl['wsT'].rearrange("p (g t) -> p g t", t=128), (), ['wsT'])
                P.dma('sp', bsf[:], Wl['bs'], (), ['bsf'])
                for g in range(8):
                    TT('pool', wsTb[:, g, :], wsT[:, g, :], U_f, ALU.mult, ['wsT', 'cf'], ['wsTb'])
                vt = [s5.T(f"vt{i}", [128, 1024], BF16) for i in range(2)]
                junk5 = s5.T("junk5", [128, 1024], BF16)
                ss5 = [s5.T(f"ss5{i}", [128, 1], F32) for i in range(2)]
                sq5 = [s5.T(f"sq5{i}", [128, 1], F32) for i in range(2)]
                rs5 = [s5.T(f"rs5{i}", [128, 1], F32) for i in range(2)]
                for tt in range(NT):
                    b = tt % 2
                    P.dma('sp', vt[b][:], sc['vb'][tt * 128:(tt + 1) * 128, :], (), [f'vt{b}'])
                    MEMSET('pool', ss5[b][:], 0.0, [f'ss5{b}'])
                    ACT(junk5[:], vt[b][:], AF.Square, [f'vt{b}', f'ss5{b}'], ['junk5', f'ss5{b}'], accum_out=ss5[b][:])
                    rstd_from(rs5[b][:], ss5[b][:], 1024, [f'ss5{b}'], [f'rs5{b}'], sq5[b][:], f'sq5{b}')
                    STT('dve', vn[:, tt, :], vt[b][:], rs5[b][:, 0:1], vgb[:], ALU.mult, ALU.mult, [f'vt{b}', f'rs5{b}', 'vgb'], [('vn', tt)])
                ut = [s5.T(f"ut{i}", [128, 512], BF16) for i in range(3)]
                zt = [s5.T(f"zt{i}", [128, 512], BF16) for i in range(3)]
                y5 = [s5.T(f"y5{i}", [128, 512], F32) for i in range(2)]
                yb5 = [s5.T(f"yb5{i}", [128, 512], BF16) for i in range(2)]
                n = 0
                its5 = [(g, tb) for g in range(8) for tb in range(NB)]

                def ld5(i):
                    g, tb = its5[i]
                    b3 = i % 3
                    P.dma('sp', ut[b3][:], sc['ub'][g * 128:(g + 1) * 128, tb * 512:(tb + 1) * 512], (), [f'ut{b3}'])
                    P.dma('sp', zt[b3][:], sc['zb'][g * 128:(g + 1) * 128, tb * 512:(tb + 1) * 512], (), [f'zt{b3}'])
                ld5(0)
                for i5, (g, tb) in enumerate(its5):
                    if True:
                        b = n % 2
                        b3 = i5 % 3
                        n += 1
                        if i5 + 1 < len(its5):
                            ld5(i5 + 1)
                        wbr_prefetch(1)
                        for j in range(4):
                            tt = tb * 4 + j
                            MM(ps[b][:, j * 128:(j + 1) * 128], vn[:, tt, g * 128:(g + 1) * 128], wsTb[:, g, :], True, False, [('vn', tt), 'wsTb'], [PK[b]])
                            MM(ps[b][:, j * 128:(j + 1) * 128], cf[0:1, 256:384], bsf[0:1, g * 128:(g + 1) * 128], False, True, ['cf', 'bsf'], [PK[b]])
                        TT('dve', y5[b][:], ps[b][:], ut[b3][:], ALU.mult, [PK[b], f'ut{b3}'], [f'y5{b}'])
                        TT('pool', yb5[b][:], y5[b][:], zt[b3][:], ALU.mult, [f'y5{b}', f'zt{b3}'], [f'yb5{b}'])
                        P.dma('act', sc['yb'][g * 128:(g + 1) * 128, tb * 512:(tb + 1) * 512], yb5[b][:], [f'yb5{b}'], [('yb', g, tb)])

            with Scope() as s6:
                P.enabled = (upto >= 6) and (6 not in skip)
                wa2f = s6.T("wa2f", [16, 512], F32)
                wa2 = s6.T("wa2", [16, 512], BF16)
                baf = s6.T("baf", [1, 512], F32)
                P.dma('sp', wa2f[:], Wl['wa2'], (), ['wa2f'])
                P.dma('sp', baf[:], Wl['ba'], (), ['baf'])
                CP('dve', wa2[:], wa2f[:], ['wa2f'], ['wa2'])
                Sf = s6.T("Sf", [128, 4, 256], F32)
                Sb = s6.T("Sb", [128, 4, 256], BF16)
                MEMSET('pool', Sf[:], 0.0, [('Sf', h) for h in range(4)])
                MEMSET('pool', Sb[:], 0.0, ['Sb'])
                mk = lambda nm, shp, dt, n: [s6.T(f"{nm}{i}", shp, dt) for i in range(n)]
                qT4 = mk("qT4", [128, 4, 128], BF16, 2)
                kT4 = mk("kT4", [128, 4, 128], BF16, 2)
                ktok = mk("ktok", [128, 512], BF16, 2)
                vtok = mk("vtok", [128, 1024], BF16, 2)
                art = mk("art", [16, 128], BF16, 2)
                zc8 = mk("zc8", [128, 8, 128], BF16, 2)
                e1 = mk("e1", [128, 512], F32, 2)
                l1 = mk("l1", [128, 512], F32, 2)
                ebT = mk("ebT", [128, 512], F32, 4)
                enbT = mk("enbT", [128, 512], F32, 2)
                ed = mk("ed", [128, 512], F32, 2)
                qg = mk("qg", [128, 4, 128], BF16, 4)
                kg = mk("kg", [128, 4, 128], BF16, 2)
                kd = mk("kd", [128, 512], BF16, 4)
                aT = mk("aT", [128, 4, 128], BF16, 2)
                sq6 = mk("sq6", [128, 8, 128], F32, 2)
                tm6 = mk("tm6", [128, 512], F32, 2)
                rs6 = mk("rs6", [128, 512], F32, 2)
                yn6 = mk("yn6", [128, 8, 128], F32, 2)
                yc6 = mk("yc6", [128, 8, 128], BF16, 2)
                qcv = sc['qc'].rearrange("(h p) s -> p h s", p=128)
                kcv = sc['kc'].rearrange("(h p) s -> p h s", p=128)
                zcv = sc['zc'].rearrange("(m p) s -> p m s", p=128)
                ycv = sc['yc'].rearrange("(m p) s -> p m s", p=128)
                ok = lambda c: 0 <= c < NT

                def L_art(c):
                    if ok(c):
                        P.dma('sp', art[c % 2][:], sc['ar'][:, c * 128:(c + 1) * 128], (), [f'art{c % 2}'])

                def L_qk(c):
                    if ok(c):
                        b = c % 2
                        cs = slice(c * 128, (c + 1) * 128)
                        P.dma('sp', qT4[b][:], qcv[:, :, cs], (), [f'qT4{b}'])
                        P.dma('sp', kT4[b][:], kcv[:, :, cs], (), [f'kT4{b}'])
                        P.dma('sp', ktok[b][:], sc['kctok'][cs, :], (), [f'ktok{b}'])

                def L_v(c):
                    if ok(c):
                        P.dma('sp', vtok[c % 2][:], sc['vc'][c * 128:(c + 1) * 128, :], (), [f'vtok{c % 2}'])

                def L_z(c):
                    if ok(c):
                        P.dma('sp', zc8[c % 2][:], zcv[:, :, c * 128:(c + 1) * 128], (), [f'zc8{c % 2}'])

                def F1(c):
                    if not ok(c):
                        return
                    b = c % 2
                    MM(ps[0][:], art[b][:], wa2[:], True, False, [f'art{b}', 'wa2'], [PK[0]])
                    MM(ps[0][:], cf[0:1, 256:384], baf[:], False, True, ['cf', 'baf'], [PK[0]])
                    ACT(e1[b][:], ps[0][:], AF.Exp, [PK[0]], [f'e1{b}'], scale=-1.0)
                    ACT(l1[b][:], e1[b][:], AF.Ln, [f'e1{b}'], [f'l1{b}'], bias=1.0)

                def F2(c):
                    if not ok(c):
                        return
                    b, b4 = c % 2, c % 4
                    for h in range(4):
                        MM(ps[1][:, h * 128:(h + 1) * 128], l1[b][:, h * 128:(h + 1) * 128], U_f, True, True, [f'l1{b}', 'cf'], [PK[1]])
                    MM(ps[2][:], Urev_f, l1[b][:], True, True, [f'l1{b}', 'cf'], [PK[2]])
                    ACT(ebT[b4][:], ps[1][:], AF.Exp, [PK[1]], [f'ebT{b4}'], scale=-1.0 / 16)
                    ACT(enbT[b][:], ps[1][:], AF.Exp, [PK[1]], [f'enbT{b}'], scale=1.0 / 16)
                    ACT(ed[b][:], ps[2][:], AF.Exp, [PK[2]], [f'ed{b}'], scale=-1.0 / 16)
                    STT('dve', qg[b4][:], qT4[b][:], 128 ** -0.5, ebT[b4][:].rearrange("p (h t) -> p h t", t=128), ALU.mult, ALU.mult,
                        [f'qT4{b}', f'ebT{b4}'], [f'qg{b4}'])
                    TT('pool', kg[b][:], kT4[b][:], enbT[b][:].rearrange("p (h t) -> p h t", t=128), ALU.mult, [f'kT4{b}', f'enbT{b}'], [f'kg{b}'])
                    TT('pool', kd[b4][:], ktok[b][:], ed[b][:], ALU.mult, [f'ktok{b}', f'ed{b}'], [f'kd{b4}'])

                def F3(c):
                    if not ok(c):
                        return
                    b, b4 = c % 2, c % 4
                    for h in range(4):
                        MM(ps[3][:, h * 128:(h + 1) * 128], kg[b][:, h, :], qg[b4][:, h, :], True, True, [f'kg{b}', f'qg{b4}'], [PK[3]])
                    TT('dve', aT[b][:], ps[3][:].rearrange("p (h t) -> p h t", t=128), U4_f.rearrange("p (h t) -> p h t", t=128), ALU.mult,
                       [PK[3], 'cf'], [f'aT{b}'])

                def B1(c):
                    if not ok(c):
                        return
                    b, b4 = c % 2, c % 4
                    for jj in range(2):
                        for h in range(4):
                            o_ = ps[4 + jj][:, h * 128:(h + 1) * 128]
                            MM(o_, vtok[b][:, h * 256 + jj * 128:h * 256 + (jj + 1) * 128], aT[b][:, h, :], True, False, [f'vtok{b}', f'aT{b}'], [PK[4 + jj]])
                            MM(o_, Sb[:, h, jj * 128:(jj + 1) * 128], qg[b4][:, h, :], False, True, ['Sb', f'qg{b4}'], [PK[4 + jj]])
                    for h in range(4):
                        pS = ps[6 + h % 2]
                        MM(pS[:, 0:256], kd[b4][:, h * 128:(h + 1) * 128], vtok[b][:, h * 256:(h + 1) * 256], True, True, [f'kd{b4}', f'vtok{b}'], [PK[6 + h % 2]])
                        TS('dve', Sf[:, h, :], Sf[:, h, :], ebT[b4][:, h * 128 + 127:h * 128 + 128], None, ALU.mult, None, [('Sf', h), f'ebT{b4}'], [('Sf', h)])
                        TT('dve', Sf[:, h, :], pS[:, 0:256], Sf[:, h, :], ALU.add, [('Sf', h), PK[6 + h % 2]], [('Sf', h)])
                    CP('act', Sb[:], Sf[:], [('Sf', h) for h in range(4)], ['Sb'])
                    for jj in range(2):
                        ACT(sq6[b][:].rearrange("p (h j) t -> p h j t", j=2)[:, :, jj, :], ps[4 + jj][:].rearrange("p (h t) -> p h t", t=128), AF.Square,
                            [PK[4 + jj]], [f'sq6{b}'])

                def B2(c):
                    if not ok(c):
                        return
                    b = c % 2
                    for h in range(4):
                        MM(ps[0][:, h * 128:(h + 1) * 128], ones_f, sq6[b][:, 2 * h, :], True, False, ['cf', f'sq6{b}'], [PK[0]])
                        MM(ps[0][:, h * 128:(h + 1) * 128], ones_f, sq6[b][:, 2 * h + 1, :], False, True, ['cf', f'sq6{b}'], [PK[0]])
                    rstd_from(rs6[b][:], ps[0][:], 256, [PK[0]], [f'rs6{b}'], tm6[b][:], f'tm6{b}')
                    for jj in range(2):
                        STT('dve', yn6[b][:].rearrange("p (h j) t -> p h j t", j=2)[:, :, jj, :], ps[4 + jj][:].rearrange("p (h t) -> p h t", t=128),
                            vecs[:, V_OG + jj:V_OG + jj + 1], rs6[b][:].rearrange("p (h t) -> p h t", t=128), ALU.mult, ALU.mult,
                            [PK[4 + jj], 'vecs', f'rs6{b}'], [f'yn6{b}'])
                    TT('pool', yc6[b][:], yn6[b][:], zc8[b][:], ALU.mult, [f'yn6{b}', f'zc8{b}'], [f'yc6{b}'])
                    P.dma('sp', ycv[:, :, c * 128:(c + 1) * 128], yc6[b][:], [f'yc6{b}'], [('yc', c)])

                for i in range(-4, NT + 1):
                    wbr_prefetch(-(-len(wbr_jobs) // max(1, NT + 1 - i)))
                    L_art(i + 4)
                    L_qk(i + 3)
                    L_v(i + 1)
                    L_z(i)
                    F1(i + 3)
                    F2(i + 2)
                    F3(i + 1)
                    B2(i - 1)
                    B1(i)

            with Scope() as s7:
                P.enabled = (upto >= 7) and (7 not in skip)
                yT = [[s7.T(f"yT{i}_{bb}", [128, 8, 512], BF16) for bb in range(3)] for i in range(2)]
                g3 = [s7.T(f"g3{i}", [128, 3, 512], BF16) for i in range(3)]
                mm_ = [[s7.T(f"mm{i}_{bb}", [128, 512], F32) for bb in range(3)] for i in range(2)]
                mo = [s7.T(f"mo{i}", [128, 512], BF16) for i in range(2)]
                gv = sc['g'].rearrange("(b c p) s -> p b c s", b=3, p=128)
                n = 0

                def ldy(tb):
                    for bb, nm in enumerate(('ya', 'yb', 'yc')):
                        P.dma('sp', yT[tb % 2][bb][:], sc[nm].rearrange("(k p) s -> p k s", p=128)[:, :, tb * 512:(tb + 1) * 512], (), [f'yT{tb % 2}_{bb}'])

                def ldg(i):
                    tb_, c_ = divmod(i, 16)
                    P.dma('sp', g3[i % 3][:], gv[:, :, c_, tb_ * 512:(tb_ + 1) * 512], (), [f'g3{i % 3}'])
                ldy(0)
                ldg(0)
                for tb in range(NB):
                    yb_ = tb % 2
                    sl = slice(tb * 512, (tb + 1) * 512)
                    for c in range(16):
                        b = n % 2
                        g3b = n % 3
                        if n + 1 < NB * 16:
                            ldg(n + 1)
                        if c == 2 and tb + 1 < NB:
                            ldy(tb + 1)
                        n += 1
                        for bb in range(3):
                            pb = (b * 3 + bb)
                            for kc in range(8):
                                MM(ps[pb][:], wbr[:, bb, kc, c * 128:(c + 1) * 128], yT[yb_][bb][:, kc, :], kc == 0, kc == 7,
                                   [('wbr', c // 4), f'yT{yb_}_{bb}'], [PK[pb]])
                            TT('dve', mm_[b][bb][:], ps[pb][:], g3[g3b][:, bb, :], ALU.mult, [PK[pb], f'g3{g3b}'], [f'mm{b}_{bb}'])
                        TT('pool', mm_[b][0][:], mm_[b][0][:], mm_[b][1][:], ALU.add, [f'mm{b}_0', f'mm{b}_1'], [f'mm{b}_0'])
                        TT('pool', mo[b][:], mm_[b][0][:], mm_[b][2][:], ALU.add, [f'mm{b}_0', f'mm{b}_2'], [f'mo{b}'])
                        P.dma('act', sc['mT'][c * 128:(c + 1) * 128, sl], mo[b][:], [f'mo{b}'], [('mT', c, tb)])

            P.enabled = True
            sW.__exit__(None, None, None)
            with Scope() as s8:
                P.enabled = (upto >= 8) and (8 not in skip)
                wo = s8.T("wo", [128, 16, D], BF16)
                wov = Wl['wout'].rearrange("p (k n) -> p k n", k=16)
                wst = [s8.T(f"wst{i}", [128, D], F32) for i in range(2)]
                for kc in range(16):
                    P.dma('sp', wst[kc % 2][:], wov[:, kc, :], (), [f'wst{kc % 2}'])
                    CP('act', wo[:, kc, :], wst[kc % 2][:], [f'wst{kc % 2}'], [('wo', kc)])
                mT = [s8.T(f"mTt{i}", [128, 16, 512], BF16) for i in range(2)]
                xr = [s8.T(f"xr{i}", [128, D], F32) for i in range(2)]
                xo = [s8.T(f"xo{i}", [128, D], F32) for i in range(2)]
                mTv = sc['mT'].rearrange("(k p) s -> p k s", p=128)
                n = 0
                for tb in range(NB):
                    mb = tb % 2
                    if tb == 0:
                        P.dma('sp', mT[0][:], mTv[:, :, 0:512], (), ['mTt0'])
                    if tb == 0:
                        P.dma('sp', xr[0][:], x_src[0:128, :], [('xd', 0)], ['xr0'])
                    if tb + 1 < NB:
                        P.dma('sp', mT[(tb + 1) % 2][:], mTv[:, :, (tb + 1) * 512:(tb + 2) * 512], (), [f'mTt{(tb + 1) % 2}'])
                    for j in range(4):
                        tt = tb * 4 + j
                        b = tt % 2
                        if tt + 1 < NT:
                            P.dma('sp', xr[(tt + 1) % 2][:], x_src[(tt + 1) * 128:(tt + 2) * 128, :], [('xd', tt + 1)], [f'xr{(tt + 1) % 2}'])
                        for cbk in range(4):
                            pb = n % 6
                            n += 1
                            for kc in range(16):
                                MM(ps[pb][:], mT[mb][:, kc, j * 128:(j + 1) * 128], wo[:, kc, cbk * 512:(cbk + 1) * 512], kc == 0, kc == 15,
                                   [f'mTt{mb}', ('wo', kc)], [PK[pb]])
                            TT('dve', xo[b][:, cbk * 512:(cbk + 1) * 512], ps[pb][:], xr[b][:, cbk * 512:(cbk + 1) * 512], ALU.add,
                               [PK[pb], f'xr{b}'], [f'xo{b}'])
                        P.dma('act', x_dst[tt * 128:(tt + 1) * 128, :], xo[b][:], [f'xo{b}'], [('xo_d', tt)])
            P.enabled = True
            ls.__exit__(None, None, None)
        gs.__exit__(None, None, None)
        P.barrier()
        P.emit()
    return nc


def _kmajor(w, cw_cols):
    K = w.shape[0]
    sub = w[:, cw_cols]
    return np.ascontiguousarray(sub.reshape(K // 128, 128, sub.shape[1]).transpose(1, 0, 2)).reshape(128, -1)


def prep_layer(inp, l):
    f = np.float32
    w_in = inp['w_in'][l]
    win = np.concatenate([_kmajor(w_in, cols) for (_, _, cols, _, _) in CHUNKS], axis=1)
    wuq = inp['mla_w_uq'][l]
    wukv = inp['mla_w_ukv'][l]
    uq_parts, ukv_parts = [], []
    for h in range(NH):
        b0 = h * 192
        uq_parts += [_kmajor(wuq, np.arange(b0, b0 + 128)), _kmajor(wuq, np.arange(b0 + 128, b0 + 192)),
                     _kmajor(wuq, np.concatenate([np.arange(b0 + 160, b0 + 192), np.arange(b0 + 128, b0 + 160)]))]
        c0 = h * 256
        ukv_parts += [_kmajor(wukv, np.arange(c0, c0 + 128)), _kmajor(wukv, np.arange(c0 + 128, c0 + 256))]
    vecs = np.zeros((128, NVEC), f)
    vecs[:, V_GT:V_GT + 16] = inp['norm_g'][l].reshape(16, 128).T
    vecs[:, V_CQG:V_CQG + 4] = inp['mla_cq_norm'][l].reshape(4, 128).T
    vecs[:, V_CKVG:V_CKVG + 4] = inp['mla_ckv_norm'][l].reshape(4, 128).T
    qg, kg = inp['mla_q_norm'][l], inp['mla_k_norm'][l]
    vecs[:, V_GQN] = qg[0:128]
    vecs[0:64, V_GQA] = qg[128:192]
    vecs[0:64, V_GQB] = np.concatenate([qg[160:192], qg[128:160]])
    vecs[:, V_GKN] = kg[0:128]
    vecs[0:64, V_GKA] = kg[128:192]
    vecs[0:64, V_GKB] = np.concatenate([kg[160:192], kg[128:160]])
    vecs[:, V_OG:V_OG + 2] = inp['gla_o_norm'][l].reshape(2, 128).T
    wbr = inp['w_branch'][l]
    wbr_r = np.ascontiguousarray(wbr.reshape(3, 8, 128, D).transpose(2, 0, 1, 3)).reshape(128, -1)
    wout_r = np.ascontiguousarray(inp['w_out'][l].reshape(16, 128, D).transpose(1, 0, 2)).reshape(128, -1)
    return {
        f"win{l}": np.ascontiguousarray(win, f),
        f"wuq{l}": np.ascontiguousarray(np.concatenate(uq_parts, axis=1), f),
        f"wukv{l}": np.ascontiguousarray(np.concatenate(ukv_parts, axis=1), f),
        f"vecs{l}": vecs,
        f"vgb{l}": np.ascontiguousarray(np.broadcast_to(inp['sgu_v_norm'][l][None, :], (128, 1024)), f),
        f"wsT{l}": np.ascontiguousarray(inp['sgu_w_s'][l].transpose(2, 0, 1).reshape(128, 1024), f),
        f"bs{l}": np.ascontiguousarray(inp['sgu_b_s'][l].reshape(1, 1024), f),
        f"wa2{l}": np.ascontiguousarray(inp['gla_w_a2'][l], f),
        f"ba{l}": np.ascontiguousarray(inp['gla_b_a'][l].reshape(1, 512), f),
        f"wbr{l}": np.ascontiguousarray(wbr_r, f),
        f"wout{l}": np.ascontiguousarray(wout_r, f),
    }


def consts():
    f = np.float32
    s = np.arange(128)
    U = (s[:, None] <= s[None, :]).astype(f)
    Urev = (s[:, None] > s[None, :]).astype(f)
    ones = np.ones((128, 128), f)
    misc = np.zeros((128, 4), f)
    inv_freq = (1.0 / (10000.0 ** (np.arange(0, 64, 2, dtype=np.float32) / np.float32(64)))).astype(f)
    misc[0:64, 0] = np.concatenate([inv_freq, inv_freq])
    misc[0:64, 1] = np.concatenate([-np.ones(32, f), np.ones(32, f)])
    cf = np.concatenate([U, Urev, ones, np.tile(U, (1, 4)), misc], axis=1)
    cb = np.concatenate([np.eye(128, dtype=f), U, ones], axis=1).astype(ml_dtypes.bfloat16)
    return np.ascontiguousarray(cf, f), np.ascontiguousarray(cb)


_NC_CACHE = {}


def run(inp, S, depth, n_cores):
    key = (S, depth)
    if key not in _NC_CACHE:
        _NC_CACHE[key] = build(S, depth)
    nc = _NC_CACHE[key]
    cf, cb = consts()
    shared = {"cf": cf, "cb": cb}
    for l in range(depth):
        shared.update(prep_layer(inp, l))
    in_maps = []
    for b in range(n_cores):
        m = dict(shared)
        m["x"] = np.ascontiguousarray(inp['x'][b], np.float32)
        m["posr"] = np.ascontiguousarray(np.broadcast_to(inp['positions'][b][None, :], (64, S)), np.int32)
        in_maps.append(m)
    res = run_bass_kernel_spmd(nc, in_maps, core_ids=list(range(n_cores)))
    return np.stack([res.results[b]["out"] for b in range(n_cores)], axis=0)


def kernel(**inputs):
    inp = {k: np.asarray(v) for k, v in inputs.items()}
    return run(inp, 4096, DEPTH, 8).astype(np.float32)
```

```python
import os
import numpy as np
import ml_dtypes
from contextlib import ExitStack
import concourse.bass as bass
import concourse.mybir as mybir
from concourse.bass_utils import run_bass_kernel_spmd

F32 = mybir.dt.float32
BF16 = mybir.dt.bfloat16
I32 = mybir.dt.int32
AF = mybir.ActivationFunctionType
ALU = mybir.AluOpType

D = 2048
DEPTH = 2
NH = 8
EPS = 1e-6
IN_COLS = 14416
TWO_PI = 6.283185
GCUT = int(os.environ.get('GCUT', '99'))
HS_ENG = os.environ.get('HS_ENG', 'dve')

ENGS = ['pe', 'act', 'dve', 'pool', 'sp']
EPOCH = 24000
NDMA = 12


class Prog:
    def __init__(self, nc, stack):
        self.nc = nc
        self.stack = stack
        self.lists = {e: [] for e in ENGS}
        self.cnt = {e: 0 for e in ENGS}
        self.cursem = {}
        self.allsems = []
        for e in ENGS:
            self.cursem[e] = self._newsem(f"c_{e}")
        self.known = {e: {} for e in ENGS}
        self.lastw = {}
        self.readers = {}
        self.dmasems = {}
        self.dmacnt = {}
        self.dmaidx = {}
        for q in ('sp', 'act', 'pool'):
            self.dmasems[q] = [self._newsem(f"d_{q}{i}") for i in range(NDMA)]
            self.dmacnt[q] = [0] * NDMA
            self.dmaidx[q] = 0
        self.outstanding = {}
        self.enabled = True

    def _newsem(self, name):
        s = self.stack.enter_context(self.nc.semaphore(name + f"_{len(self.allsems)}"))
        self.allsems.append(s)
        return s

    def _need(self, eng, reads, writes, extra=()):
        need = {}

        def add(tok):
            if tok is None:
                return
            s, v = tok
            if need.get(s, 0) < v:
                need[s] = v
        for r in reads:
            add(self.lastw.get(r))
        for w in writes:
            add(self.lastw.get(w))
            for s, v in self.readers.get(w, {}).items():
                add((s, v))
        for t in extra:
            add(t)
        waits = []
        kn = self.known[eng]
        for s, v in need.items():
            if eng == 'pe' and s is self.cursem['pe']:
                continue
            if kn.get(s, 0) >= v:
                continue
            kn[s] = v
            waits.append((s, v))
        return waits

    def _commit(self, tok, reads, writes):
        s, v = tok
        for r in reads:
            d = self.readers.setdefault(r, {})
            if d.get(s, 0) < v:
                d[s] = v
        for w in writes:
            self.lastw[w] = tok
            self.readers[w] = {}
        self.outstanding[s] = v

    def op(self, eng, fn, reads=(), writes=()):
        if not self.enabled:
            return None
        waits = self._need(eng, reads, writes)
        if self.cnt[eng] >= EPOCH:
            self.cursem[eng] = self._newsem(f"c_{eng}")
            self.cnt[eng] = 0
        self.cnt[eng] += 1
        tok = (self.cursem[eng], self.cnt[eng])
        self.lists[eng].append((waits, fn, (tok[0], 1)))
        self._commit(tok, reads, writes)
        return tok

    def dma(self, q, out, in_, reads=(), writes=()):
        if not self.enabled:
            return None
        i = self.dmaidx[q]
        self.dmaidx[q] = (i + 1) % NDMA
        sem = self.dmasems[q][i]
        prev = self.dmacnt[q][i]
        extra = [(sem, prev)] if prev > 0 else []
        waits = self._need(q, reads, writes, extra)
        if prev + 16 > EPOCH:
            sem = self._newsem(f"d_{q}{i}")
            self.dmasems[q][i] = sem
            prev = 0
        self.dmacnt[q][i] = prev + 16
        tok = (sem, prev + 16)

        def fn(e, out=out, in_=in_):
            return e.dma_start(out=out, in_=in_)
        self.lists[q].append((waits, fn, (sem, 16)))
        self._commit(tok, reads, writes)
        return tok

    def barrier(self):
        for e in ENGS:
            kn = self.known[e]
            waits = []
            for s, v in self.outstanding.items():
                if kn.get(s, 0) >= v:
                    continue
                kn[s] = v
                waits.append((s, v))
            if waits:
                self.lists[e].append((waits, None, None))
        self.lastw = {}
        self.readers = {}

    def emit(self):
        nc = self.nc
        engmap = {'pe': 'tensor', 'act': 'scalar', 'dve': 'vector', 'pool': 'gpsimd', 'sp': 'sync'}
        with nc.Block() as block:
            for e in ENGS:
                lst = self.lists[e]

                def body(eng, lst=lst):
                    for waits, fn, inc in lst:
                        for s, v in waits:
                            eng.wait_ge(s, v)
                        if fn is not None:
                            ins = fn(eng)
                            ins.then_inc(inc[0], inc[1])
                getattr(block, engmap[e])(body)


def win_chunks():
    ch = []
    def fm(name, c0, n, cw=128):
        for i in range(n // cw):
            ch.append((name, i, np.arange(c0 + i * cw, c0 + (i + 1) * cw), cw, 'fm'))
    def tm(name, c0, n):
        for i in range(n // 512):
            ch.append((name, i, np.arange(c0 + i * 512, c0 + (i + 1) * 512), 512, 'tm'))
    fm('cq', 0, 512)
    fm('ckv', 512, 512)
    ch.append(('krA', 0, np.arange(1024, 1088), 64, 'fm'))
    ch.append(('krB', 0, np.concatenate([np.arange(1056, 1088), np.arange(1024, 1056)]), 64, 'fm'))
    ch.append(('ar', 0, np.arange(7232, 7248), 16, 'fm'))
    tm('kctok', 5696, 512)
    fm('qc', 5184, 512)
    fm('kc', 5696, 512)
    ch.append(('vc', 0, np.arange(6208, 6720), 512, 'tm'))
    fm('za', 1088, 1024)
    ch.append(('vc', 1, np.arange(6720, 7232), 512, 'tm'))
    fm('zb', 4160, 1024)
    ch.append(('vb', 0, np.arange(3136, 3648), 512, 'tm'))
    fm('zc', 7248, 1024)
    ch.append(('vb', 1, np.arange(3648, 4160), 512, 'tm'))
    fm('ub', 2112, 1024)
    fm('g', 8272, 6144)
    return ch


CHUNKS = win_chunks()
WIN_R_COLS = 16 * sum(c[3] for c in CHUNKS)

V_GT, V_CQG, V_CKVG, V_GQN, V_GQA, V_GQB, V_GKN, V_GKA, V_GKB, V_OG, NVEC = 0, 16, 20, 24, 25, 26, 27, 28, 29, 30, 32


def build(S, depth, upto=99, skip=()):
    NT = S // 128
    NB = S // 512
    nc = bass.Bass("TRN2", target_bir_lowering=False)
    dram_in = lambda name, shape, dt: nc.dram_tensor(name, shape, dt, kind="ExternalInput").ap()
    dram_sc = lambda name, shape, dt: nc.dram_tensor(name, shape, dt).ap()
    x_in = dram_in("x", [S, D], F32)
    posr = dram_in("posr", [64, S], I32)
    cf_d = dram_in("cf", [128, 128 * 3 + 512 + 4], F32)
    cb_d = dram_in("cb", [128, 384], BF16)
    W = []
    for l in range(depth):
        W.append(dict(
            win=dram_in(f"win{l}", [128, WIN_R_COLS], F32),
            wuq=dram_in(f"wuq{l}", [128, NH * 1024], F32),
            wukv=dram_in(f"wukv{l}", [128, NH * 1024], F32),
            vecs=dram_in(f"vecs{l}", [128, NVEC], F32),
            vgb=dram_in(f"vgb{l}", [128, 1024], F32),
            wsT=dram_in(f"wsT{l}", [128, 1024], F32),
            bs=dram_in(f"bs{l}", [1, 1024], F32),
            wa2=dram_in(f"wa2{l}", [16, 512], F32),
            ba=dram_in(f"ba{l}", [1, 512], F32),
            wbr=dram_in(f"wbr{l}", [128, 3 * 8 * D], F32),
            wout=dram_in(f"wout{l}", [128, 16 * D], F32),
        ))
    out_d = nc.dram_tensor("out", [S, D], F32, kind="ExternalOutput").ap()
    xmid = dram_sc("xmid", [S, D], F32)
    sc = dict(
        cq=dram_sc("s_cq", [512, S], BF16), ckv=dram_sc("s_ckv", [512, S], BF16),
        krA=dram_sc("s_krA", [64, S], BF16), krB=dram_sc("s_krB", [64, S], BF16),
        ar=dram_sc("s_ar", [16, S], BF16),
        qc=dram_sc("s_qc", [512, S], BF16), kc=dram_sc("s_kc", [512, S], BF16),
        kctok=dram_sc("s_kctok", [S, 512], BF16), vc=dram_sc("s_vc", [S, 1024], BF16),
        za=dram_sc("s_za", [1024, S], BF16), zb=dram_sc("s_zb", [1024, S], BF16), zc=dram_sc("s_zc", [1024, S], BF16),
        ub=dram_sc("s_ub", [1024, S], BF16), vb=dram_sc("s_vb", [S, 1024], BF16),
        g=dram_sc("s_g", [6144, S], BF16),
        ya=dram_sc("s_ya", [1024, S], BF16), yb=dram_sc("s_yb", [1024, S], BF16), yc=dram_sc("s_yc", [1024, S], BF16),
        mT=dram_sc("s_mT", [D, S], BF16),
    )

    with ExitStack() as st:
        P = Prog(nc, st)

        def MM(out, lhsT, rhs, s, e, r, w):
            P.op('pe', lambda en: en.matmul(out, lhsT, rhs, start=s, stop=e), r, w)

        def ACT(out, in_, func, r, w, **kw):
            P.op('act', lambda en: en.activation(out, in_, func, **kw), r, w)

        def TT(eng, out, a, b, op, r, w):
            P.op(eng, lambda en: en.tensor_tensor(out, a, b, op), r, w)

        def TS(eng, out, a, s1, s2, op0, op1, r, w):
            if s2 is None:
                P.op(eng, lambda en: en.tensor_scalar(out, a, s1, None, op0), r, w)
            else:
                P.op(eng, lambda en: en.tensor_scalar(out, a, s1, s2, op0, op1), r, w)

        def STT(eng, out, a, s, b, op0, op1, r, w):
            P.op(eng, lambda en: en.scalar_tensor_tensor(out, a, s, b, op0, op1), r, w)

        def CP(eng, out, a, r, w):
            if eng == 'act':
                P.op(eng, lambda en: en.activation(out, a, AF.Copy), r, w)
            else:
                P.op(eng, lambda en: en.tensor_copy(out, a), r, w)

        def MEMSET(eng, out, val, w):
            P.op(eng, lambda en: en.memset(out, val), (), w)

        def RECIP(out, a, r, w):
            P.op('dve', lambda en: en.reciprocal(out, a), r, w)

        def rstd_from(out, ssum, n, r, w, tmp, tmpk):
            ACT(tmp, ssum, AF.Ln, list(r), [tmpk], scale=1.0 / n, bias=EPS)
            ACT(out, tmp, AF.Exp, [tmpk], list(w), scale=-0.5)

        uid = [0]

        class Scope:
            def __init__(self):
                self.es = ExitStack()
            def __enter__(self):
                self.es.__enter__()
                return self
            def __exit__(self, *a):
                P.barrier()
                return self.es.__exit__(*a)
            def T(self, name, shape, dt):
                uid[0] += 1
                return self.es.enter_context(nc.sbuf_tensor(f"t{uid[0]}_{name}", shape, dt))

        gs = Scope()
        gs.__enter__()
        cf = gs.T("cf", [128, 128 * 3 + 512 + 4], F32)
        cb = gs.T("cbt", [128, 384], BF16)
        P.dma('sp', cf[:], cf_d, (), ['cf'])
        P.dma('sp', cb[:], cb_d, (), ['cb'])
        U_f = cf[:, 0:128]
        Urev_f = cf[:, 128:256]
        ones_f = cf[:, 256:384]
        U4_f = cf[:, 384:896]
        invf2 = cf[0:64, 896:897]
        sgn = cf[0:64, 897:898]
        ident_b = cb[:, 0:128]
        U_b = cb[:, 128:256]
        ones_b = cb[:, 256:384]
        ps = [st.enter_context(nc.psum_tensor(f"ps{i}", [128, 512], F32)) for i in range(8)]
        ptv = [ps[6][:].bitcast(BF16), ps[7][:].bitcast(BF16)]
        PK = [f"ps{i}" for i in range(8)]

        def rope_tables(cos2, sin2):
            s0 = Scope()
            s0.__enter__()
            pi_t = s0.T("pos_i", [64, S], I32)
            u_t = s0.T("rp_u", [64, S], F32)
            r_i = s0.T("rp_ri", [64, S], I32)
            r_f = s0.T("rp_rf", [64, S], F32)
            m_t = s0.T("rp_m", [64, S], F32)
            P.dma('sp', pi_t[:], posr, (), ['pos_i'])
            CP('dve', u_t[:], pi_t[:], ['pos_i'], ['u'])
            TS('dve', u_t[:], u_t[:], invf2, 1.0 / (2 * np.pi), ALU.mult, ALU.mult, ['u', 'cf'], ['u'])
            for which, dst in (('sin', sin2), ('cos', cos2)):
                if which == 'cos':
                    TS('dve', u_t[:], u_t[:], 0.25, None, ALU.add, None, ['u'], ['u'])
                CP('dve', r_i[:], u_t[:], ['u'], ['ri'])
                CP('dve', r_f[:], r_i[:], ['ri'], ['rf'])
                TT('dve', r_f[:], u_t[:], r_f[:], ALU.subtract, ['u', 'rf'], ['rf'])
                TS('dve', m_t[:], r_f[:], 0.5, None, ALU.is_gt, None, ['rf'], ['m'])
                TT('dve', r_f[:], r_f[:], m_t[:], ALU.subtract, ['rf', 'm'], ['rf'])
                TS('dve', m_t[:], r_f[:], -0.5, None, ALU.is_lt, None, ['rf'], ['m'])
                TT('dve', r_f[:], r_f[:], m_t[:], ALU.add, ['rf', 'm'], ['rf'])
                ACT(dst[:], r_f[:], AF.Sin, ['rf'], [which], scale=TWO_PI)
            TS('dve', sin2[:], sin2[:], sgn, None, ALU.mult, None, ['sin', 'cf'], ['sin'])
            en_ = P.enabled
            P.enabled = True
            s0.__exit__(None, None, None)
            P.enabled = en_

        for l in range(depth):
            Wl = W[l]
            x_src = x_in if l == 0 else xmid
            x_dst = out_d if l == depth - 1 else xmid
            ls = Scope()
            ls.__enter__()
            vecs = ls.T("vecs", [128, NVEC], F32)
            P.dma('sp', vecs[:], Wl['vecs'], (), ['vecs'])

            with Scope() as s12:
                hT = s12.T("hT", [128, 16, S], BF16)
                with Scope() as s1:
                    P.enabled = (upto >= 1) and (1 not in skip)
                    xt = [s1.T(f"xt{i}", [128, D], F32) for i in range(2)]
                    hs = [s1.T(f"hs{i}", [128, D], BF16) for i in range(2)]
                    junk = s1.T("junk", [128, D], BF16)
                    ss = [s1.T(f"ss{i}", [128, 1], F32) for i in range(2)]
                    sq = [s1.T(f"sq{i}", [128, 1], F32) for i in range(2)]
                    rs = [s1.T(f"rs{i}", [128, 1], F32) for i in range(2)]
                    G = s1.T("G", [128, 16, 128], F32)
                    for kc in range(16):
                        TS('dve', G[:, kc, :], ones_f, vecs[:, V_GT + kc:V_GT + kc + 1], None, ALU.mult, None, ['cf', 'vecs'], ['G'])
                    P.dma('sp', xt[0][:], x_src[0:128, :], [('xd', 0)], ['xt0'])
                    for tt in range(NT):
                        b = tt % 2
                        if tt + 1 < NT:
                            P.dma('sp', xt[(tt + 1) % 2][:], x_src[(tt + 1) * 128:(tt + 2) * 128, :], [('xd', tt + 1)], [f'xt{(tt + 1) % 2}'])
                        MEMSET('pool', ss[b][:], 0.0, [f'ss{b}'])
                        ACT(junk[:], xt[b][:], AF.Square, [f'xt{b}', f'ss{b}'], ['junk', f'ss{b}'], accum_out=ss[b][:])
                        rstd_from(rs[b][:], ss[b][:], D, [f'ss{b}'], [f'rs{b}'], sq[b][:], f'sq{b}')
                        TS('dve', hs[b][:, 0:1024], xt[b][:, 0:1024], rs[b][:, 0:1], None, ALU.mult, None, [f'xt{b}', f'rs{b}'], [f'hs{b}a'])
                        TS(HS_ENG, hs[b][:, 1024:2048], xt[b][:, 1024:2048], rs[b][:, 0:1], None, ALU.mult, None, [f'xt{b}', f'rs{b}'], [f'hs{b}b'])
                        for kq in range(4):
                            half = (tt * 4 + kq) % 2
                            for j in range(4):
                                kc = kq * 4 + j
                                P.op('pe', (lambda o, i: (lambda en: en.transpose(o, i, ident_b)))(
                                    ptv[half][:, j * 128:(j + 1) * 128], hs[b][:, kc * 128:(kc + 1) * 128]),
                                    [f'hs{b}a' if kq < 2 else f'hs{b}b', 'cb'], [PK[6 + half]])
                            TT('dve', hT[:, kq * 4:(kq + 1) * 4, tt * 128:(tt + 1) * 128], ptv[half][:, 0:512].rearrange("p (j t) -> p j t", t=128),
                               G[:, kq * 4:(kq + 1) * 4, :], ALU.mult, [PK[6 + half], 'G'], [('hT', tt)])
                with Scope() as s2:
                    P.enabled = (upto >= 2) and (2 not in skip)
                    wf = [s2.T(f"wf{i}", [128, 16, 128], BF16) for i in range(3)]
                    wt = [s2.T(f"wt{i}", [128, 16, 512], BF16) for i in range(1)]
                    ob = [s2.T(f"ob{i}", [128, 512], BF16) for i in range(4)]
                    nfm = 0
                    ntm = 0
                    nps = 0
                    nob = 0
                    off = 0
                    for (name, idx, cols, cw, mode) in CHUNKS:
                        src = Wl['win'][:, 16 * off:16 * (off + cw)].rearrange("p (k c) -> p k c", c=cw)
                        off += cw
                        if mode == 'fm':
                            wb = nfm % 3
                            nfm += 1
                            P.dma('pool', wf[wb][:, :, 0:cw], src, (), [f'wf{wb}'])
                            func = {'za': AF.Silu, 'zb': AF.Silu, 'zc': AF.Silu, 'ub': AF.Gelu_apprx_tanh, 'g': AF.Sigmoid}.get(name)
                            for tb in range(NB):
                                pb = nps % 6
                                nps += 1
                                for kc in range(16):
                                    MM(ps[pb][0:cw, :], wf[wb][:, kc, 0:cw], hT[:, kc, tb * 512:(tb + 1) * 512], kc == 0, kc == 15,
                                       [f'wf{wb}'] + [('hT', tb * 4 + i) for i in range(4)], [PK[pb]])
                                o = nob % 4
                                nob += 1
                                if func is not None:
                                    ACT(ob[o][0:cw, :], ps[pb][0:cw, :], func, [PK[pb]], [f'ob{o}'])
                                else:
                                    CP('dve', ob[o][0:cw, :], ps[pb][0:cw, :], [PK[pb]], [f'ob{o}'])
                                P.dma('sp', sc[name][idx * cw:(idx + 1) * cw, tb * 512:(tb + 1) * 512], ob[o][0:cw, :], [f'ob{o}'], [(name, idx, tb)])
                        else:
                            wb = 0
                            ntm += 1
                            P.dma('pool', wt[wb][:], src, (), [f'wt{wb}'])
                            func = AF.Gelu_apprx_tanh if name == 'vb' else None
                            for tt in range(NT):
                                pb = nps % 6
                                nps += 1
                                for kc in range(16):
                                    MM(ps[pb][:], hT[:, kc, tt * 128:(tt + 1) * 128], wt[wb][:, kc, :], kc == 0, kc == 15,
                                       [f'wt{wb}', ('hT', tt)], [PK[pb]])
                                o = nob % 4
                                nob += 1
                                if func is not None:
                                    ACT(ob[o][:], ps[pb][:], func, [PK[pb]], [f'ob{o}'])
                                else:
                                    CP('dve', ob[o][:], ps[pb][:], [PK[pb]], [f'ob{o}'])
                                P.dma('sp', sc[name][tt * 128:(tt + 1) * 128, idx * 512:(idx + 1) * 512], ob[o][:], [f'ob{o}'], [(name, idx, tt)])

            with Scope() as s3:
                P.enabled = (upto >= 3) and (3 not in skip)
                cos2 = s3.T("cos2", [64, S], F32)
                sin2 = s3.T("sin2", [64, S], F32)
                rope_tables(cos2, sin2)
                cqn = s3.T("cqn", [128, 4, S], BF16)
                ckvn = s3.T("ckvn", [128, 4, S], BF16)
                krA = s3.T("krA", [64, S], BF16)
                krB = s3.T("krB", [64, S], BF16)
                P.dma('sp', krA[:], sc['krA'], (), ['krA'])
                P.dma('sp', krB[:], sc['krB'], (), ['krB'])
                tmpa = [s3.T(f"tmpa{i}", [128, 512], F32) for i in range(2)]
                rst = [s3.T(f"rst{i}", [128, 512], F32) for i in range(2)]
                s3p = Scope()
                s3p.__enter__()
                sqt = [s3p.T(f"sqt{i}", [128, 4, 512], BF16) for i in range(2)]
                cin = [s3p.T(f"cin{i}", [128, 4, 512], BF16) for i in range(2)]
                n = 0
                for (nm, dst, gcol) in (('cq', cqn, V_CQG), ('ckv', ckvn, V_CKVG)):
                    srcv = sc[nm].rearrange("(k p) s -> p k s", p=128)
                    for tb in range(NB):
                        b = n % 2
                        n += 1
                        P.dma('sp', cin[b][:], srcv[:, :, tb * 512:(tb + 1) * 512], (), [f'cin{b}'])
                        ACT(sqt[b][:], cin[b][:], AF.Square, [f'cin{b}'], [f'sqt{b}'])
                        for kc in range(4):
                            MM(ps[b][:], ones_b, sqt[b][:, kc, :], kc == 0, kc == 3, ['cb', f'sqt{b}'], [PK[b]])
                        rstd_from(rst[b][:], ps[b][:], 512, [PK[b]], [f'rst{b}'], tmpa[b][:], f'tmpa{b}')
                        for kc in range(4):
                            STT('dve', dst[:, kc, tb * 512:(tb + 1) * 512], cin[b][:, kc, :], vecs[:, gcol + kc:gcol + kc + 1], rst[b][:],
                                ALU.mult, ALU.mult, [f'cin{b}', 'vecs', f'rst{b}'], [(nm + 'n', tb)])
                en_ = P.enabled
                P.enabled = True
                s3p.__exit__(None, None, None)
                P.enabled = en_
                Qn = s3.T("Qn", [128, S], BF16)
                Qr = s3.T("Qr", [64, S], BF16)
                Kn = s3.T("Kn", [128, S], BF16)
                Kr = s3.T("Kr", [64, S], BF16)
                Vh = s3.T("Vh", [128, NT, 128], BF16)
                wq = s3.T("wq", [128, 1024], BF16)
                wkv = s3.T("wkv", [128, 1024], BF16)
                sqn = [s3.T(f"sqn{i}", [128, 512], BF16) for i in range(4)]
                sqa = [s3.T(f"sqa{i}", [64, 512], BF16) for i in range(4)]
                Ar = [s3.T(f"Ar{i}", [64, 512], F32) for i in range(2)]
                Br = [s3.T(f"Br{i}", [64, 512], F32) for i in range(2)]
                An = [s3.T(f"An{i}", [64, 512], F32) for i in range(2)]
                Bn = [s3.T(f"Bn{i}", [64, 512], F32) for i in range(2)]
                PT = [s3.T(f"PT{i}", [128, 512], BF16) for i in range(4)]
                zat = [s3.T(f"zat{i}", [128, 512], BF16) for i in range(2)]
                rL = [s3.T("rL0", [128, 512], F32)] * 2
                yf = [s3.T(f"yf{i}", [128, 512], F32) for i in range(2)]
                yo = [s3.T(f"yo{i}", [128, 512], BF16) for i in range(2)]
                SCALE = 192 ** -0.5
                it = 0
                for h in range(NH):
                    P.dma('pool', wq[:], Wl['wuq'][:, h * 1024:(h + 1) * 1024], (), ['wq'])
                    P.dma('pool', wkv[:], Wl['wukv'][:, h * 1024:(h + 1) * 1024], (), ['wkv'])
                    wq_n = lambda kc: wq[:, kc * 128:(kc + 1) * 128]
                    wq_a = lambda kc: wq[:, 512 + kc * 64:512 + (kc + 1) * 64]
                    wq_b = lambda kc: wq[:, 768 + kc * 64:768 + (kc + 1) * 64]
                    wk_n = lambda kc: wkv[:, kc * 128:(kc + 1) * 128]
                    wv = lambda kc: wkv[:, 512 + kc * 128:512 + (kc + 1) * 128]
                    def Pst(tb):
                        sl = slice(tb * 512, (tb + 1) * 512)
                        par = tb % 2
                        pqn, kqn = ps[0 + par], PK[0 + par]
                        pkn, kkn = ps[2 + par], PK[2 + par]
                        for kc in range(4):
                            MM(pqn[:], wq_n(kc), cqn[:, kc, sl], kc == 0, kc == 3, ['wq', ('cqn', tb)], [kqn])
                        for kc in range(4):
                            MM(ps[4][0:64, :], wq_a(kc), cqn[:, kc, sl], kc == 0, kc == 3, ['wq', ('cqn', tb)], [PK[4]])
                        for kc in range(4):
                            MM(ps[5][0:64, :], wq_b(kc), cqn[:, kc, sl], kc == 0, kc == 3, ['wq', ('cqn', tb)], [PK[5]])
                        for kc in range(4):
                            MM(pkn[:], wk_n(kc), ckvn[:, kc, sl], kc == 0, kc == 3, ['wkv', ('ckvn', tb)], [kkn])
                        for j in range(4):
                            tt = tb * 4 + j
                            for kc in range(4):
                                MM(ps[6][:, j * 128:(j + 1) * 128], ckvn[:, kc, tt * 128:(tt + 1) * 128], wv(kc), kc == 0, kc == 3,
                                   ['wkv', ('ckvn', tb)], [PK[6]])
                        bq_, bk_ = par * 2, par * 2 + 1
                        ACT(sqn[bq_][:], pqn[:], AF.Square, [kqn], [f'sqn{bq_}'])
                        ACT(sqa[bq_][:], ps[4][0:64, :], AF.Square, [PK[4]], [f'sqa{bq_}'])
                        ACT(Ar[par][:], ps[4][0:64, :], AF.Copy, [PK[4]], [f'Ar{par}'])
                        ACT(Br[par][:], ps[5][0:64, :], AF.Copy, [PK[5]], [f'Br{par}'])
                        ACT(sqn[bk_][:], pkn[:], AF.Square, [kkn], [f'sqn{bk_}'])
                        ACT(sqa[bk_][:], krA[:, sl], AF.Square, ['krA'], [f'sqa{bk_}'])
                        CP('act', Vh[:, tb * 4:(tb + 1) * 4, :], ps[6][:].rearrange("p (j d) -> p j d", d=128), [PK[6]], [('Vh', tb)])

                    def Sst(tb):
                        sl = slice(tb * 512, (tb + 1) * 512)
                        par = tb % 2
                        pqn, kqn = ps[0 + par], PK[0 + par]
                        pkn, kkn = ps[2 + par], PK[2 + par]
                        bq_, bk_ = par * 2, par * 2 + 1
                        b = 0
                        MM(ps[7][:], ones_b, sqn[bq_][:], True, False, ['cb', f'sqn{bq_}'], [PK[7]])
                        MM(ps[7][:], cb[0:64, 256:384], sqa[bq_][:], False, True, ['cb', f'sqa{bq_}'], [PK[7]])
                        rstd_from(rst[b][:], ps[7][:], 192, [PK[7]], [f'rst{b}'], tmpa[b][:], f'tmpa{b}')
                        STT('dve', Qn[:, sl], pqn[:], vecs[:, V_GQN:V_GQN + 1], rst[b][:], ALU.mult, ALU.mult, [kqn, 'vecs', f'rst{b}'], [('Qn', tb)])
                        STT('dve', An[b][:], Ar[par][:], vecs[0:64, V_GQA:V_GQA + 1], rst[b][0:64, :], ALU.mult, ALU.mult, [f'Ar{par}', 'vecs', f'rst{b}'], [f'An{b}'])
                        STT('dve', Bn[b][:], Br[par][:], vecs[0:64, V_GQB:V_GQB + 1], rst[b][0:64, :], ALU.mult, ALU.mult, [f'Br{par}', 'vecs', f'rst{b}'], [f'Bn{b}'])
                        TT('pool', An[b][:], An[b][:], cos2[:, sl], ALU.mult, [f'An{b}', 'cos'], [f'An{b}'])
                        TT('pool', Bn[b][:], Bn[b][:], sin2[:, sl], ALU.mult, [f'Bn{b}', 'sin'], [f'Bn{b}'])
                        TT('pool', Qr[:, sl], An[b][:], Bn[b][:], ALU.add, [f'An{b}', f'Bn{b}'], [('Qr', tb)])
                        b = 1
                        MM(ps[7][:], ones_b, sqn[bk_][:], True, False, ['cb', f'sqn{bk_}'], [PK[7]])
                        MM(ps[7][:], cb[0:64, 256:384], sqa[bk_][:], False, True, ['cb', f'sqa{bk_}'], [PK[7]])
                        rstd_from(rst[b][:], ps[7][:], 192, [PK[7]], [f'rst{b}'], tmpa[b][:], f'tmpa{b}')
                        STT('dve', Kn[:, sl], pkn[:], vecs[:, V_GKN:V_GKN + 1], rst[b][:], ALU.mult, ALU.mult, [kkn, 'vecs', f'rst{b}'], [('Kn', tb)])
                        STT('dve', An[b][:], krA[:, sl], vecs[0:64, V_GKA:V_GKA + 1], rst[b][0:64, :], ALU.mult, ALU.mult, ['krA', 'vecs', f'rst{b}'], [f'An{b}'])
                        STT('dve', Bn[b][:], krB[:, sl], vecs[0:64, V_GKB:V_GKB + 1], rst[b][0:64, :], ALU.mult, ALU.mult, ['krB', 'vecs', f'rst{b}'], [f'Bn{b}'])
                        TT('pool', An[b][:], An[b][:], cos2[:, sl], ALU.mult, [f'An{b}', 'cos'], [f'An{b}'])
                        TT('pool', Bn[b][:], Bn[b][:], sin2[:, sl], ALU.mult, [f'Bn{b}', 'sin'], [f'Bn{b}'])
                        TT('pool', Kr[:, sl], An[b][:], Bn[b][:], ALU.add, [f'An{b}', f'Bn{b}'], [('Kr', tb)])
                    Pst(0)
                    for tb in range(NB):
                        if tb + 1 < NB:
                            Pst(tb + 1)
                        Sst(tb)
                    for qb in range(NB):
                        ob_ = qb % 2
                        pO, pL = ps[3 + ob_ * 2], ps[4 + ob_ * 2]
                        kO, kL = PK[3 + ob_ * 2], PK[4 + ob_ * 2]
                        P.dma('sp', zat[ob_][:], sc['za'][h * 128:(h + 1) * 128, qb * 512:(qb + 1) * 512], (), [f'zat{ob_}'])
                        nk = 4 * qb + 4
                        SB = [0, 1, 2, 7]

                        def s_part(kt):
                            j = max(0, kt - 4 * qb)
                            c0 = j * 128
                            sb_ = kt % 4
                            pS, kS = ps[SB[sb_]], PK[SB[sb_]]
                            qs = slice(qb * 512 + c0, (qb + 1) * 512)
                            ks = slice(kt * 128, (kt + 1) * 128)
                            MM(pS[:, c0:512], Kn[:, ks], Qn[:, qs], True, False, [('Kn', kt // 4), ('Qn', qb)], [kS])
                            MM(pS[:, c0:512], Kr[:, ks], Qr[:, qs], False, True, [('Kr', kt // 4), ('Qr', qb)], [kS])
                            ACT(PT[sb_][:, c0:512], pS[:, c0:512], AF.Exp, [kS], [f'PT{sb_}'], scale=SCALE)
                            if kt >= 4 * qb:
                                TT('pool', PT[sb_][:, c0:c0 + 128], PT[sb_][:, c0:c0 + 128], U_b, ALU.mult, [f'PT{sb_}', 'cb'], [f'PT{sb_}'])

                        def pv_part(kt):
                            j = max(0, kt - 4 * qb)
                            c0 = j * 128
                            sb_ = kt % 4
                            MM(pO[:, c0:512], Vh[:, kt, :], PT[sb_][:, c0:512], kt == 0, kt == nk - 1, [('Vh', kt // 4), f'PT{sb_}'], [kO])
                            MM(pL[:, c0:512], ones_b, PT[sb_][:, c0:512], kt == 0, kt == nk - 1, ['cb', f'PT{sb_}'], [kL])
                        LOOK = 3
                        for kt in range(nk + LOOK):
                            if kt < nk:
                                s_part(kt)
                            if kt >= LOOK:
                                pv_part(kt - LOOK)
                        RECIP(rL[0][:], pL[:], [kL], ['rL0'])
                        TT('dve', yf[ob_][:], pO[:], rL[0][:], ALU.mult, [kO, 'rL0'], [f'yf{ob_}'])
                        TT('pool', yo[ob_][:], yf[ob_][:], zat[ob_][:], ALU.mult, [f'yf{ob_}', f'zat{ob_}'], [f'yo{ob_}'])
                        P.dma('sp', sc['ya'][h * 128:(h + 1) * 128, qb * 512:(qb + 1) * 512], yo[ob_][:], [f'yo{ob_}'], [('ya', h, qb)])

            sW = Scope()
            sW.__enter__()
            wbr = sW.T("wbr", [128, 3, 8, D], BF16)
            wbv = Wl['wbr'].rearrange("p (b k n) -> p b k n", b=3, k=8)
            wbr_jobs = [(cg, bb, kc) for cg in range(4) for bb in range(3) for kc in range(8)]

            def wbr_prefetch(n_):
                en_ = P.enabled
                P.enabled = (upto >= 7) and (7 not in skip)
                for _ in range(n_):
                    if wbr_jobs:
                        cg, bb, kc = wbr_jobs.pop(0)
                        P.dma('pool', wbr[:, bb, kc, cg * 512:(cg + 1) * 512], wbv[:, bb, kc, cg * 512:(cg + 1) * 512], (), [('wbr', cg)])
                P.enabled = en_
            with Scope() as s5:
                P.enabled = (upto >= 5) and (5 not in skip)
                vn = s5.T("vn", [128, NT, 1024], BF16)
                vgb = s5.T("vgb", [128, 1024], F32)
                wsT = s5.T("wsT", [128, 8, 128], F32)
                wsTb = s5.T("wsTb", [128, 8, 128], BF16)
                bsf = s5.T("bsf", [1, 1024], F32)
                P.dma('sp', vgb[:], Wl['vgb'], (), ['vgb'])
                P.dma('sp', wsT[:], Wl['wsT'].rearrange("p (g t) -> p g t", t=128), (), ['wsT'])
                P.dma('sp', bsf[:], Wl['bs'], (), ['bsf'])
                for g in range(8):
                    TT('pool', wsTb[:, g, :], wsT[:, g, :], U_f, ALU.mult, ['wsT', 'cf'], ['wsTb'])
                vt = [s5.T(f"vt{i}", [128, 1024], BF16) for i in range(4)]
                junk5 = s5.T("junk5", [128, 1024], BF16)
                ss5 = [s5.T(f"ss5{i}", [128, 1], F32) for i in range(4)]
                sq5 = [s5.T(f"sq5{i}", [128, 1], F32) for i in range(4)]
                rs5 = [s5.T(f"rs5{i}", [128, 1], F32) for i in range(4)]
                for tt in range(NT):
                    b = tt % 4
                    P.dma('sp', vt[b][:], sc['vb'][tt * 128:(tt + 1) * 128, :], (), [f'vt{b}'])
                    MEMSET('pool', ss5[b][:], 0.0, [f'ss5{b}'])
                    ACT(junk5[:], vt[b][:], AF.Square, [f'vt{b}', f'ss5{b}'], ['junk5', f'ss5{b}'], accum_out=ss5[b][:])
                    rstd_from(rs5[b][:], ss5[b][:], 1024, [f'ss5{b}'], [f'rs5{b}'], sq5[b][:], f'sq5{b}')
                    STT('dve', vn[:, tt, :], vt[b][:], rs5[b][:, 0:1], vgb[:], ALU.mult, ALU.mult, [f'vt{b}', f'rs5{b}', 'vgb'], [('vn', tt)])
                ut = [s5.T(f"ut{i}", [128, 512], BF16) for i in range(3)]
                zt = [s5.T(f"zt{i}", [128, 512], BF16) for i in range(3)]
                y5 = [s5.T(f"y5{i}", [128, 512], F32) for i in range(2)]
                yb5 = [s5.T(f"yb5{i}", [128, 512], BF16) for i in range(2)]
                n = 0
                its5 = [(g, tb) for g in range(8) for tb in range(NB)]

                def ld5(i):
                    g, tb = its5[i]
                    b3 = i % 3
                    P.dma('sp', ut[b3][:], sc['ub'][g * 128:(g + 1) * 128, tb * 512:(tb + 1) * 512], (), [f'ut{b3}'])
                    P.dma('sp', zt[b3][:], sc['zb'][g * 128:(g + 1) * 128, tb * 512:(tb + 1) * 512], (), [f'zt{b3}'])
                ld5(0)
                for i5, (g, tb) in enumerate(its5):
                    if True:
                        b = n % 2
                        b3 = i5 % 3
                        n += 1
                        if i5 + 1 < len(its5):
                            ld5(i5 + 1)
                        wbr_prefetch(1)
                        for j in range(4):
                            tt = tb * 4 + j
                            MM(ps[b][:, j * 128:(j + 1) * 128], vn[:, tt, g * 128:(g + 1) * 128], wsTb[:, g, :], True, False, [('vn', tt), 'wsTb'], [PK[b]])
                            MM(ps[b][:, j * 128:(j + 1) * 128], cf[0:1, 256:384], bsf[0:1, g * 128:(g + 1) * 128], False, True, ['cf', 'bsf'], [PK[b]])
                        TT('dve', y5[b][:], ps[b][:], ut[b3][:], ALU.mult, [PK[b], f'ut{b3}'], [f'y5{b}'])
                        TT('pool', yb5[b][:], y5[b][:], zt[b3][:], ALU.mult, [f'y5{b}', f'zt{b3}'], [f'yb5{b}'])
                        P.dma('act', sc['yb'][g * 128:(g + 1) * 128, tb * 512:(tb + 1) * 512], yb5[b][:], [f'yb5{b}'], [('yb', g, tb)])

            with Scope() as s6:
                P.enabled = (upto >= 6) and (6 not in skip)
                wa2f = s6.T("wa2f", [16, 512], F32)
                wa2 = s6.T("wa2", [16, 512], BF16)
                baf = s6.T("baf", [1, 512], F32)
                P.dma('sp', wa2f[:], Wl['wa2'], (), ['wa2f'])
                P.dma('sp', baf[:], Wl['ba'], (), ['baf'])
                CP('dve', wa2[:], wa2f[:], ['wa2f'], ['wa2'])
                Sf = s6.T("Sf", [128, 4, 256], F32)
                Sb = s6.T("Sb", [128, 4, 256], BF16)
                MEMSET('pool', Sf[:], 0.0, [('Sf', h) for h in range(4)])
                MEMSET('pool', Sb[:], 0.0, ['Sb'])
                mk = lambda nm, shp, dt, n: [s6.T(f"{nm}{i}", shp, dt) for i in range(n)]
                qT4 = mk("qT4", [128, 4, 128], BF16, 2)
                kT4 = mk("kT4", [128, 4, 128], BF16, 2)
                ktok = mk("ktok", [128, 512], BF16, 2)
                vtok = mk("vtok", [128, 1024], BF16, 2)
                art = mk("art", [16, 128], BF16, 2)
                zc8 = mk("zc8", [128, 8, 128], BF16, 2)
                e1 = mk("e1", [128, 512], F32, 2)
                l1 = mk("l1", [128, 512], F32, 2)
                ebT = mk("ebT", [128, 512], F32, 4)
                enbT = mk("enbT", [128, 512], F32, 2)
                ed = mk("ed", [128, 512], F32, 2)
                qg = mk("qg", [128, 4, 128], BF16, 4)
                kg = mk("kg", [128, 4, 128], BF16, 2)
                kd = mk("kd", [128, 512], BF16, 4)
                aT = mk("aT", [128, 4, 128], BF16, 2)
                sq6 = mk("sq6", [128, 8, 128], F32, 2)
                tm6 = mk("tm6", [128, 512], F32, 2)
                rs6 = mk("rs6", [128, 512], F32, 2)
                yn6 = mk("yn6", [128, 8, 128], F32, 2)
                yc6 = mk("yc6", [128, 8, 128], BF16, 2)
                qcv = sc['qc'].rearrange("(h p) s -> p h s", p=128)
                kcv = sc['kc'].rearrange("(h p) s -> p h s", p=128)
                zcv = sc['zc'].rearrange("(m p) s -> p m s", p=128)
                ycv = sc['yc'].rearrange("(m p) s -> p m s", p=128)
                ok = lambda c: 0 <= c < NT

                def L_art(c):
                    if ok(c):
                        P.dma('sp', art[c % 2][:], sc['ar'][:, c * 128:(c + 1) * 128], (), [f'art{c % 2}'])

                def L_qk(c):
                    if ok(c):
                        b = c % 2
                        cs = slice(c * 128, (c + 1) * 128)
                        P.dma('sp', qT4[b][:], qcv[:, :, cs], (), [f'qT4{b}'])
                        P.dma('sp', kT4[b][:], kcv[:, :, cs], (), [f'kT4{b}'])
                        P.dma('sp', ktok[b][:], sc['kctok'][cs, :], (), [f'ktok{b}'])

                def L_v(c):
                    if ok(c):
                        P.dma('sp', vtok[c % 2][:], sc['vc'][c * 128:(c + 1) * 128, :], (), [f'vtok{c % 2}'])

                def L_z(c):
                    if ok(c):
                        P.dma('sp', zc8[c % 2][:], zcv[:, :, c * 128:(c + 1) * 128], (), [f'zc8{c % 2}'])

                def F1(c):
                    if not ok(c):
                        return
                    b = c % 2
                    MM(ps[0][:], art[b][:], wa2[:], True, False, [f'art{b}', 'wa2'], [PK[0]])
                    MM(ps[0][:], cf[0:1, 256:384], baf[:], False, True, ['cf', 'baf'], [PK[0]])
                    ACT(e1[b][:], ps[0][:], AF.Exp, [PK[0]], [f'e1{b}'], scale=-1.0)
                    ACT(l1[b][:], e1[b][:], AF.Ln, [f'e1{b}'], [f'l1{b}'], bias=1.0)

                def F2(c):
                    if not ok(c):
                        return
                    b, b4 = c % 2, c % 4
                    for h in range(4):
                        MM(ps[1][:, h * 128:(h + 1) * 128], l1[b][:, h * 128:(h + 1) * 128], U_f, True, True, [f'l1{b}', 'cf'], [PK[1]])
                    MM(ps[2][:], Urev_f, l1[b][:], True, True, [f'l1{b}', 'cf'], [PK[2]])
                    ACT(ebT[b4][:], ps[1][:], AF.Exp, [PK[1]], [f'ebT{b4}'], scale=-1.0 / 16)
                    ACT(enbT[b][:], ps[1][:], AF.Exp, [PK[1]], [f'enbT{b}'], scale=1.0 / 16)
                    ACT(ed[b][:], ps[2][:], AF.Exp, [PK[2]], [f'ed{b}'], scale=-1.0 / 16)
                    STT('dve', qg[b4][:], qT4[b][:], 128 ** -0.5, ebT[b4][:].rearrange("p (h t) -> p h t", t=128), ALU.mult, ALU.mult,
                        [f'qT4{b}', f'ebT{b4}'], [f'qg{b4}'])
                    TT('pool', kg[b][:], kT4[b][:], enbT[b][:].rearrange("p (h t) -> p h t", t=128), ALU.mult, [f'kT4{b}', f'enbT{b}'], [f'kg{b}'])
                    TT('pool', kd[b4][:], ktok[b][:], ed[b][:], ALU.mult, [f'ktok{b}', f'ed{b}'], [f'kd{b4}'])

                def F3(c):
                    if not ok(c):
                        return
                    b, b4 = c % 2, c % 4
                    for h in range(4):
                        MM(ps[3][:, h * 128:(h + 1) * 128], kg[b][:, h, :], qg[b4][:, h, :], True, True, [f'kg{b}', f'qg{b4}'], [PK[3]])
                    TT('dve', aT[b][:], ps[3][:].rearrange("p (h t) -> p h t", t=128), U4_f.rearrange("p (h t) -> p h t", t=128), ALU.mult,
                       [PK[3], 'cf'], [f'aT{b}'])

                def B1(c):
                    if not ok(c):
                        return
                    b, b4 = c % 2, c % 4
                    for jj in range(2):
                        for h in range(4):
                            o_ = ps[4 + jj][:, h * 128:(h + 1) * 128]
                            MM(o_, vtok[b][:, h * 256 + jj * 128:h * 256 + (jj + 1) * 128], aT[b][:, h, :], True, False, [f'vtok{b}', f'aT{b}'], [PK[4 + jj]])
                            MM(o_, Sb[:, h, jj * 128:(jj + 1) * 128], qg[b4][:, h, :], False, True, ['Sb', f'qg{b4}'], [PK[4 + jj]])
                    for h in range(4):
                        pS = ps[6 + h % 2]
                        MM(pS[:, 0:256], kd[b4][:, h * 128:(h + 1) * 128], vtok[b][:, h * 256:(h + 1) * 256], True, True, [f'kd{b4}', f'vtok{b}'], [PK[6 + h % 2]])
                        TS('dve', Sf[:, h, :], Sf[:, h, :], ebT[b4][:, h * 128 + 127:h * 128 + 128], None, ALU.mult, None, [('Sf', h), f'ebT{b4}'], [('Sf', h)])
                        TT('dve', Sf[:, h, :], pS[:, 0:256], Sf[:, h, :], ALU.add, [('Sf', h), PK[6 + h % 2]], [('Sf', h)])
                    CP('act', Sb[:], Sf[:], [('Sf', h) for h in range(4)], ['Sb'])
                    for jj in range(2):
                        ACT(sq6[b][:].rearrange("p (h j) t -> p h j t", j=2)[:, :, jj, :], ps[4 + jj][:].rearrange("p (h t) -> p h t", t=128), AF.Square,
                            [PK[4 + jj]], [f'sq6{b}'])

                def B2(c):
                    if not ok(c):
                        return
                    b = c % 2
                    for h in range(4):
                        MM(ps[0][:, h * 128:(h + 1) * 128], ones_f, sq6[b][:, 2 * h, :], True, False, ['cf', f'sq6{b}'], [PK[0]])
                        MM(ps[0][:, h * 128:(h + 1) * 128], ones_f, sq6[b][:, 2 * h + 1, :], False, True, ['cf', f'sq6{b}'], [PK[0]])
                    rstd_from(rs6[b][:], ps[0][:], 256, [PK[0]], [f'rs6{b}'], tm6[b][:], f'tm6{b}')
                    for jj in range(2):
                        STT('dve', yn6[b][:].rearrange("p (h j) t -> p h j t", j=2)[:, :, jj, :], ps[4 + jj][:].rearrange("p (h t) -> p h t", t=128),
                            vecs[:, V_OG + jj:V_OG + jj + 1], rs6[b][:].rearrange("p (h t) -> p h t", t=128), ALU.mult, ALU.mult,
                            [PK[4 + jj], 'vecs', f'rs6{b}'], [f'yn6{b}'])
                    TT('pool', yc6[b][:], yn6[b][:], zc8[b][:], ALU.mult, [f'yn6{b}', f'zc8{b}'], [f'yc6{b}'])
                    P.dma('sp', ycv[:, :, c * 128:(c + 1) * 128], yc6[b][:], [f'yc6{b}'], [('yc', c)])

                for i in range(-4, NT + 1):
                    wbr_prefetch(-(-len(wbr_jobs) // max(1, NT + 1 - i)))
                    L_art(i + 4)
                    L_qk(i + 3)
                    L_v(i + 1)
                    L_z(i)
                    F1(i + 3)
                    F2(i + 2)
                    F3(i + 1)
                    B2(i - 1)
                    B1(i)

            with Scope() as s7:
                P.enabled = (upto >= 7) and (7 not in skip)
                yT = [[s7.T(f"yT{i}_{bb}", [128, 8, 512], BF16) for bb in range(3)] for i in range(2)]
                g3 = [s7.T(f"g3{i}", [128, 3, 512], BF16) for i in range(3)]
                mm_ = [[s7.T(f"mm{i}_{bb}", [128, 512], F32) for bb in range(3)] for i in range(2)]
                mo = [s7.T(f"mo{i}", [128, 512], BF16) for i in range(2)]
                gv = sc['g'].rearrange("(b c p) s -> p b c s", b=3, p=128)
                n = 0

                def ldy(tb):
                    for bb, nm in enumerate(('ya', 'yb', 'yc')):
                        P.dma('sp', yT[tb % 2][bb][:], sc[nm].rearrange("(k p) s -> p k s", p=128)[:, :, tb * 512:(tb + 1) * 512], (), [f'yT{tb % 2}_{bb}'])

                def ldg(i):
                    tb_, c_ = divmod(i, 16)
                    P.dma('sp', g3[i % 3][:], gv[:, :, c_, tb_ * 512:(tb_ + 1) * 512], (), [f'g3{i % 3}'])
                ldy(0)
                ldg(0)
                for tb in range(NB):
                    yb_ = tb % 2
                    sl = slice(tb * 512, (tb + 1) * 512)
                    for c in range(16):
                        b = n % 2
                        g3b = n % 3
                        if n + 1 < NB * 16:
                            ldg(n + 1)
                        if c == 2 and tb + 1 < NB:
                            ldy(tb + 1)
                        n += 1
                        for bb in range(3):
                            pb = (b * 3 + bb)
                            for kc in range(8):
                                MM(ps[pb][:], wbr[:, bb, kc, c * 128:(c + 1) * 128], yT[yb_][bb][:, kc, :], kc == 0, kc == 7,
                                   [('wbr', c // 4), f'yT{yb_}_{bb}'], [PK[pb]])
                            TT('dve', mm_[b][bb][:], ps[pb][:], g3[g3b][:, bb, :], ALU.mult, [PK[pb], f'g3{g3b}'], [f'mm{b}_{bb}'])
                        TT('pool', mm_[b][0][:], mm_[b][0][:], mm_[b][1][:], ALU.add, [f'mm{b}_0', f'mm{b}_1'], [f'mm{b}_0'])
                        TT('pool', mo[b][:], mm_[b][0][:], mm_[b][2][:], ALU.add, [f'mm{b}_0', f'mm{b}_2'], [f'mo{b}'])
                        P.dma('act', sc['mT'][c * 128:(c + 1) * 128, sl], mo[b][:], [f'mo{b}'], [('mT', c, tb)])

            P.enabled = True
            sW.__exit__(None, None, None)
            with Scope() as s8:
                P.enabled = (upto >= 8) and (8 not in skip)
                wo = s8.T("wo", [128, 16, D], BF16)
                wov = Wl['wout'].rearrange("p (k n) -> p k n", k=16)
                wst = [s8.T(f"wst{i}", [128, D], F32) for i in range(2)]
                for kc in range(16):
                    P.dma('sp', wst[kc % 2][:], wov[:, kc, :], (), [f'wst{kc % 2}'])
                    CP('act', wo[:, kc, :], wst[kc % 2][:], [f'wst{kc % 2}'], [('wo', kc)])
                mT = [s8.T(f"mTt{i}", [128, 16, 512], BF16) for i in range(2)]
                xr = [s8.T(f"xr{i}", [128, D], F32) for i in range(2)]
                xo = [s8.T(f"xo{i}", [128, D], F32) for i in range(2)]
                mTv = sc['mT'].rearrange("(k p) s -> p k s", p=128)
                n = 0
                for tb in range(NB):
                    mb = tb % 2
                    if tb == 0:
                        P.dma('sp', mT[0][:], mTv[:, :, 0:512], (), ['mTt0'])
                    if tb == 0:
                        P.dma('sp', xr[0][:], x_src[0:128, :], [('xd', 0)], ['xr0'])
                    if tb + 1 < NB:
                        P.dma('sp', mT[(tb + 1) % 2][:], mTv[:, :, (tb + 1) * 512:(tb + 2) * 512], (), [f'mTt{(tb + 1) % 2}'])
                    for j in range(4):
                        tt = tb * 4 + j
                        b = tt % 2
                        if tt + 1 < NT:
                            P.dma('sp', xr[(tt + 1) % 2][:], x_src[(tt + 1) * 128:(tt + 2) * 128, :], [('xd', tt + 1)], [f'xr{(tt + 1) % 2}'])
                        for cbk in range(4):
                            pb = n % 6
                            n += 1
                            for kc in range(16):
                                MM(ps[pb][:], mT[mb][:, kc, j * 128:(j + 1) * 128], wo[:, kc, cbk * 512:(cbk + 1) * 512], kc == 0, kc == 15,
                                   [f'mTt{mb}', ('wo', kc)], [PK[pb]])
                            TT('dve', xo[b][:, cbk * 512:(cbk + 1) * 512], ps[pb][:], xr[b][:, cbk * 512:(cbk + 1) * 512], ALU.add,
                               [PK[pb], f'xr{b}'], [f'xo{b}'])
                        P.dma('act', x_dst[tt * 128:(tt + 1) * 128, :], xo[b][:], [f'xo{b}'], [('xo_d', tt)])
            P.enabled = True
            ls.__exit__(None, None, None)
        gs.__exit__(None, None, None)
        P.barrier()
        P.emit()
    return nc


def _kmajor(w, cw_cols):
    K = w.shape[0]
    sub = w[:, cw_cols]
    return np.ascontiguousarray(sub.reshape(K // 128, 128, sub.shape[1]).transpose(1, 0, 2)).reshape(128, -1)


def prep_layer(inp, l):
    f = np.float32
    w_in = inp['w_in'][l]
    win = np.concatenate([_kmajor(w_in, cols) for (_, _, cols, _, _) in CHUNKS], axis=1)
    wuq = inp['mla_w_uq'][l]
    wukv = inp['mla_w_ukv'][l]
    uq_parts, ukv_parts = [], []
    for h in range(NH):
        b0 = h * 192
        uq_parts += [_kmajor(wuq, np.arange(b0, b0 + 128)), _kmajor(wuq, np.arange(b0 + 128, b0 + 192)),
                     _kmajor(wuq, np.concatenate([np.arange(b0 + 160, b0 + 192), np.arange(b0 + 128, b0 + 160)]))]
        c0 = h * 256
        ukv_parts += [_kmajor(wukv, np.arange(c0, c0 + 128)), _kmajor(wukv, np.arange(c0 + 128, c0 + 256))]
    vecs = np.zeros((128, NVEC), f)
    vecs[:, V_GT:V_GT + 16] = inp['norm_g'][l].reshape(16, 128).T
    vecs[:, V_CQG:V_CQG + 4] = inp['mla_cq_norm'][l].reshape(4, 128).T
    vecs[:, V_CKVG:V_CKVG + 4] = inp['mla_ckv_norm'][l].reshape(4, 128).T
    qg, kg = inp['mla_q_norm'][l], inp['mla_k_norm'][l]
    vecs[:, V_GQN] = qg[0:128]
    vecs[0:64, V_GQA] = qg[128:192]
    vecs[0:64, V_GQB] = np.concatenate([qg[160:192], qg[128:160]])
    vecs[:, V_GKN] = kg[0:128]
    vecs[0:64, V_GKA] = kg[128:192]
    vecs[0:64, V_GKB] = np.concatenate([kg[160:192], kg[128:160]])
    vecs[:, V_OG:V_OG + 2] = inp['gla_o_norm'][l].reshape(2, 128).T
    wbr = inp['w_branch'][l]
    wbr_r = np.ascontiguousarray(wbr.reshape(3, 8, 128, D).transpose(2, 0, 1, 3)).reshape(128, -1)
    wout_r = np.ascontiguousarray(inp['w_out'][l].reshape(16, 128, D).transpose(1, 0, 2)).reshape(128, -1)
    return {
        f"win{l}": np.ascontiguousarray(win, f),
        f"wuq{l}": np.ascontiguousarray(np.concatenate(uq_parts, axis=1), f),
        f"wukv{l}": np.ascontiguousarray(np.concatenate(ukv_parts, axis=1), f),
        f"vecs{l}": vecs,
        f"vgb{l}": np.ascontiguousarray(np.broadcast_to(inp['sgu_v_norm'][l][None, :], (128, 1024)), f),
        f"wsT{l}": np.ascontiguousarray(inp['sgu_w_s'][l].transpose(2, 0, 1).reshape(128, 1024), f),
        f"bs{l}": np.ascontiguousarray(inp['sgu_b_s'][l].reshape(1, 1024), f),
        f"wa2{l}": np.ascontiguousarray(inp['gla_w_a2'][l], f),
        f"ba{l}": np.ascontiguousarray(inp['gla_b_a'][l].reshape(1, 512), f),
        f"wbr{l}": np.ascontiguousarray(wbr_r, f),
        f"wout{l}": np.ascontiguousarray(wout_r, f),
    }


def consts():
    f = np.float32
    s = np.arange(128)
    U = (s[:, None] <= s[None, :]).astype(f)
    Urev = (s[:, None] > s[None, :]).astype(f)
    ones = np.ones((128, 128), f)
    misc = np.zeros((128, 4), f)
    inv_freq = (1.0 / (10000.0 ** (np.arange(0, 64, 2, dtype=np.float32) / np.float32(64)))).astype(f)
    misc[0:64, 0] = np.concatenate([inv_freq, inv_freq])
    misc[0:64, 1] = np.concatenate([-np.ones(32, f), np.ones(32, f)])
    cf = np.concatenate([U, Urev, ones, np.tile(U, (1, 4)), misc], axis=1)
    cb = np.concatenate([np.eye(128, dtype=f), U, ones], axis=1).astype(ml_dtypes.bfloat16)
    return np.ascontiguousarray(cf, f), np.ascontiguousarray(cb)


_NC_CACHE = {}


def run(inp, S, depth, n_cores):
    key = (S, depth)
    if key not in _NC_CACHE:
        _NC_CACHE[key] = build(S, depth)
    nc = _NC_CACHE[key]
    cf, cb = consts()
    shared = {"cf": cf, "cb": cb}
    for l in range(depth):
        shared.update(prep_layer(inp, l))
    in_maps = []
    for b in range(n_cores):
        m = dict(shared)
        m["x"] = np.ascontiguousarray(inp['x'][b], np.float32)
        m["posr"] = np.ascontiguousarray(np.broadcast_to(inp['positions'][b][None, :], (64, S)), np.int32)
        in_maps.append(m)
    res = run_bass_kernel_spmd(nc, in_maps, core_ids=list(range(n_cores)))
    return np.stack([res.results[b]["out"] for b in range(n_cores)], axis=0)


def kernel(**inputs):
    inp = {k: np.asarray(v) for k, v in inputs.items()}
    return run(inp, 4096, DEPTH, 8).astype(np.float32)
```
